# Optimizing a Trainium2 kernel written in Bass

```python
import math
import jax, jax.numpy as jnp
from jax import lax
import numpy as np

D_MODEL = 1024
BATCH = 8
SEQ = 4096
DEPTH = 1

CHUNK = 64
N_META = 16
SSD_HEADS = 16
SSD_HEAD_DIM = 64
SSD_INNER = SSD_HEADS * SSD_HEAD_DIM
SSD_GROUPS = 4
SSD_STATE = 128
SSD_CONV = 4
SSD_CONV_DIM = SSD_INNER + 2 * SSD_GROUPS * SSD_STATE
DA_HEADS = 8
DA_HEAD_DIM = 64
DA_V_DIM = 2 * DA_HEAD_DIM
DA_QK_WIDTH = DA_HEADS * 2 * DA_HEAD_DIM
DA_V_WIDTH = DA_HEADS * DA_V_DIM
Q_BLOCK = 128
N_BRANCH = 2
IN_SIZES = (SSD_INNER, SSD_CONV_DIM, SSD_HEADS, DA_QK_WIDTH, DA_QK_WIDTH, DA_V_WIDTH, N_BRANCH * D_MODEL)
IN_COLS = SSD_INNER + SSD_CONV_DIM + SSD_HEADS + 2 * DA_QK_WIDTH + DA_V_WIDTH + N_BRANCH * D_MODEL
N_EXPERTS = 32
TOP_K = 4
D_FF = D_MODEL
SWIGLU_LIMIT = 7.0
SWIGLU_ALPHA = 1.702
MOE_BLOCK = 128
DEEPNORM_ALPHA = (2.0 * DEPTH) ** 0.25
DEEPNORM_BETA = (8.0 * DEPTH) ** -0.25
LN_EPS = 1e-5
RMS_EPS = 1e-6

kernel_name = "hybrid_ssd_diffattn_moe_streaming"


def layer_norm(x, g, b):
    xf = x.astype(jnp.float32)
    mu = jnp.mean(xf, axis=-1, keepdims=True)
    var = jnp.mean(jnp.square(xf - mu), axis=-1, keepdims=True)
    return ((xf - mu) * lax.rsqrt(var + LN_EPS)).astype(x.dtype) * g + b


def split_points(sizes):
    pts, acc = [], 0
    for s in sizes[:-1]:
        acc += s
        pts.append(acc)
    return pts


def chunk_ids(pos):
    return jnp.where(pos < N_META, 0, 1 + (pos - N_META) // CHUNK)


def causal_dwconv(x, w, b):
    y = lax.conv_general_dilated(x, w[:, None, :].astype(x.dtype), window_strides=(1,),
                                 padding=((SSD_CONV - 1, 0),),
                                 dimension_numbers=("NWC", "WIO", "NWC"),
                                 feature_group_count=x.shape[-1])
    return y + b


def segsum_exp(a):
    T = a.shape[-1]
    cs = jnp.cumsum(a, axis=-1)
    diff = cs[..., :, None] - cs[..., None, :]
    mask = jnp.tril(jnp.ones((T, T), dtype=bool))
    return jnp.exp(jnp.where(mask, diff, -jnp.inf))


def ssd_mixer(z, xbc, dt_raw, conv_w, conv_b, dt_bias, a_log, d_skip, norm_w):
    f32 = jnp.float32
    Bsz, T, _ = z.shape
    xbc = jax.nn.silu(causal_dwconv(xbc, conv_w, conv_b))
    xs, Bm, Cm = jnp.split(xbc, [SSD_INNER, SSD_INNER + SSD_GROUPS * SSD_STATE], axis=-1)
    dt = jax.nn.softplus((dt_raw + dt_bias).astype(f32))
    A = -jnp.exp(a_log.astype(f32))
    front = (-N_META) % CHUNK
    back = (-(front + T)) % CHUNK
    nc = (front + T + back) // CHUNK
    r = SSD_HEADS // SSD_GROUPS

    def pad(t):
        return jnp.pad(t, ((0, 0), (front, back)) + ((0, 0),) * (t.ndim - 2))

    x_c = pad(xs.astype(f32)).reshape(Bsz, nc, CHUNK, SSD_GROUPS, r, SSD_HEAD_DIM)
    dt_c = pad(dt).reshape(Bsz, nc, CHUNK, SSD_GROUPS, r)
    B_c = pad(Bm.astype(f32)).reshape(Bsz, nc, CHUNK, SSD_GROUPS, SSD_STATE)
    C_c = pad(Cm.astype(f32)).reshape(Bsz, nc, CHUNK, SSD_GROUPS, SSD_STATE)
    xdt = x_c * dt_c[..., None]
    a_t = jnp.moveaxis(dt_c * A.reshape(SSD_GROUPS, r), 2, -1)
    a_cs = jnp.cumsum(a_t, axis=-1)
    Lmat = segsum_exp(a_t)
    cb = jnp.einsum("bclgn,bcsgn->bcgls", C_c, B_c)
    y_diag = jnp.einsum("bcgls,bcgrls,bcsgrp->bclgrp", cb, Lmat, xdt)
    decay_to_end = jnp.exp(a_cs[..., -1:] - a_cs)
    chunk_states = jnp.einsum("bclgn,bcgrl,bclgrp->bcgrpn", B_c, decay_to_end, xdt)
    chunk_decay = jnp.exp(a_cs[..., -1])

    def step(state, inp):
        s_c, d_c = inp
        return state * d_c[..., None, None] + s_c, state

    h0 = jnp.zeros((Bsz, SSD_GROUPS, r, SSD_HEAD_DIM, SSD_STATE), f32)
    _, prev = lax.scan(step, h0, (jnp.moveaxis(chunk_states, 1, 0), jnp.moveaxis(chunk_decay, 1, 0)))
    prev = jnp.moveaxis(prev, 0, 1)
    y_off = jnp.einsum("bclgn,bcgrpn,bcgrl->bclgrp", C_c, prev, jnp.exp(a_cs))
    y = (y_diag + y_off).reshape(Bsz, nc * CHUNK, SSD_INNER)[:, front:front + T]
    y = y + xs.astype(f32) * jnp.repeat(d_skip.astype(f32), SSD_HEAD_DIM)
    g = (y * jax.nn.silu(z.astype(f32))).reshape(Bsz, T, SSD_GROUPS, SSD_INNER // SSD_GROUPS)
    g = g * lax.rsqrt(jnp.mean(jnp.square(g), axis=-1, keepdims=True) + RMS_EPS)
    return (g.reshape(Bsz, T, SSD_INNER) * norm_w.astype(f32)).astype(z.dtype)


def diff_attention(q, k, v, lam_q1, lam_k1, lam_q2, lam_k2, subln_w, lambda_init):
    f32 = jnp.float32
    Bsz, T = q.shape[0], q.shape[1]
    lam = (jnp.exp(jnp.sum(lam_q1.astype(f32) * lam_k1.astype(f32)))
           - jnp.exp(jnp.sum(lam_q2.astype(f32) * lam_k2.astype(f32))) + lambda_init)
    n_blk = -(-T // Q_BLOCK)
    Tq = n_blk * Q_BLOCK
    qp = jnp.pad(q, ((0, 0), (0, Tq - T), (0, 0), (0, 0), (0, 0)))
    q_blocks = jnp.swapaxes(qp.reshape(Bsz, n_blk, Q_BLOCK, DA_HEADS, 2, DA_HEAD_DIM), 0, 1)
    q_pos = jnp.arange(Tq, dtype=jnp.int32).reshape(n_blk, Q_BLOCK)
    k_chunk = chunk_ids(jnp.arange(T, dtype=jnp.int32))
    scale = DA_HEAD_DIM ** -0.5

    def block(args):
        qb, qpos = args
        s = jnp.einsum("bqhjd,bkhjd->bhjqk", qb, k).astype(f32) * scale
        mask = k_chunk[None, :] <= chunk_ids(qpos)[:, None]
        p = jax.nn.softmax(jnp.where(mask, s, -jnp.inf), axis=-1)
        w = p[:, :, 0] - lam * p[:, :, 1]
        return jnp.einsum("bhqk,bkhe->bqhe", w.astype(v.dtype), v)

    o = lax.map(block, (q_blocks, q_pos))
    o = jnp.swapaxes(o, 0, 1).reshape(Bsz, Tq, DA_HEADS, DA_V_DIM)[:, :T].astype(f32)
    o = o * lax.rsqrt(jnp.mean(jnp.square(o), axis=-1, keepdims=True) + RMS_EPS) * subln_w.astype(f32)
    o = o * (1.0 - lambda_init)
    return o.reshape(Bsz, T, DA_V_WIDTH).astype(q.dtype)


def clamped_swiglu(hidden):
    gate, up = hidden[..., :D_FF], hidden[..., D_FF:]
    gate = jnp.minimum(gate, SWIGLU_LIMIT)
    up = jnp.clip(up, -SWIGLU_LIMIT, SWIGLU_LIMIT)
    return gate * jax.nn.sigmoid(SWIGLU_ALPHA * gate) * (up + 1.0)


def moe_ffn(x, w_router, b_router, w_gate_up, b_gate_up, w_down, b_down):
    Bsz, T, D = x.shape
    xf = x.reshape(-1, D)
    N = xf.shape[0]
    logits = (xf @ w_router + b_router).astype(jnp.float32)
    top_val, top_idx = lax.top_k(logits, TOP_K)
    gates = jax.nn.softmax(top_val, axis=-1)
    M = N * TOP_K
    e_flat = top_idx.reshape(-1).astype(jnp.int32)
    g_flat = gates.reshape(-1)
    tok_flat = jnp.arange(M, dtype=jnp.int32) // TOP_K
    order = jnp.argsort(e_flat)
    e_sorted = e_flat[order]
    counts = jnp.bincount(e_flat, length=N_EXPERTS)
    padded = (counts + MOE_BLOCK - 1) // MOE_BLOCK * MOE_BLOCK
    pad_end = jnp.cumsum(padded)
    pad_start = pad_end - padded
    grp_start = jnp.cumsum(counts) - counts
    dest = pad_start[e_sorted] + (jnp.arange(M, dtype=jnp.int32) - grp_start[e_sorted])
    n_blocks = -(-M // MOE_BLOCK) + N_EXPERTS
    S = n_blocks * MOE_BLOCK
    slot_tok = jnp.full((S,), N, jnp.int32).at[dest].set(tok_flat[order])
    slot_gate = jnp.zeros((S,), jnp.float32).at[dest].set(g_flat[order])
    blk_expert = jnp.minimum(
        jnp.searchsorted(pad_end, jnp.arange(n_blocks, dtype=pad_end.dtype) * MOE_BLOCK, side="right"),
        N_EXPERTS - 1).astype(jnp.int32)
    x_pad = jnp.concatenate([xf, jnp.zeros((1, D), xf.dtype)], axis=0)
    xs = x_pad[slot_tok].reshape(n_blocks, MOE_BLOCK, D)

    def expert_block(args):
        xb, e = args
        hidden = xb @ w_gate_up[e] + b_gate_up[e]
        return clamped_swiglu(hidden) @ w_down[e] + b_down[e]

    ys = lax.map(expert_block, (xs, blk_expert)).reshape(S, D)
    out = jax.ops.segment_sum(ys * slot_gate[:, None].astype(ys.dtype), slot_tok, num_segments=N + 1)[:N]
    return out.reshape(Bsz, T, D).astype(x.dtype)


def setup_inputs(seed: int = 0) -> dict:
    key = jax.random.key(seed)
    ks = jax.random.split(key, 40)
    f32 = jnp.float32
    nrm = lambda k, shape, s: jax.random.normal(k, shape, f32) * s
    dt = jnp.exp(jax.random.uniform(ks[8], (DEPTH, SSD_HEADS), f32) * (math.log(0.1) - math.log(0.001)) + math.log(0.001))
    return {
        "x": nrm(ks[0], (BATCH, SEQ, D_MODEL), 1.0),
        "meta_tokens": nrm(ks[1], (N_META, D_MODEL), 1.0),
        "ln_in_g": 1.0 + nrm(ks[2], (D_MODEL,), 0.02),
        "ln_in_b": nrm(ks[3], (D_MODEL,), 0.02),
        "w_in": nrm(ks[4], (DEPTH, D_MODEL, IN_COLS), D_MODEL ** -0.5),
        "b_gate": nrm(ks[5], (DEPTH, N_BRANCH * D_MODEL), 0.02),
        "conv_w": nrm(ks[6], (DEPTH, SSD_CONV, SSD_CONV_DIM), SSD_CONV ** -0.5),
        "conv_b": nrm(ks[7], (DEPTH, SSD_CONV_DIM), 0.02),
        "dt_bias": dt + jnp.log(-jnp.expm1(-dt)),
        "a_log": jnp.log(jax.random.uniform(ks[9], (DEPTH, SSD_HEADS), f32, 1.0, 16.0)),
        "d_skip": 1.0 + nrm(ks[10], (DEPTH, SSD_HEADS), 0.1),
        "ssd_norm_w": 1.0 + nrm(ks[11], (DEPTH, SSD_INNER), 0.02),
        "w_ssd_out": nrm(ks[12], (DEPTH, SSD_INNER, D_MODEL), SSD_INNER ** -0.5 * DEEPNORM_BETA),
        "lam_q1": nrm(ks[13], (DEPTH, DA_HEAD_DIM), 0.1),
        "lam_k1": nrm(ks[14], (DEPTH, DA_HEAD_DIM), 0.1),
        "lam_q2": nrm(ks[15], (DEPTH, DA_HEAD_DIM), 0.1),
        "lam_k2": nrm(ks[16], (DEPTH, DA_HEAD_DIM), 0.1),
        "subln_w": 1.0 + nrm(ks[17], (DEPTH, DA_V_DIM), 0.02),
        "w_da_out": nrm(ks[18], (DEPTH, DA_V_WIDTH, D_MODEL), DA_V_WIDTH ** -0.5 * DEEPNORM_BETA),
        "w_out": nrm(ks[19], (DEPTH, D_MODEL, D_MODEL), D_MODEL ** -0.5 * DEEPNORM_BETA),
        "ln1_g": 1.0 + nrm(ks[20], (DEPTH, D_MODEL), 0.02),
        "ln1_b": nrm(ks[21], (DEPTH, D_MODEL), 0.02),
        "w_router": nrm(ks[22], (DEPTH, D_MODEL, N_EXPERTS), D_MODEL ** -0.5),
        "b_router": nrm(ks[23], (DEPTH, N_EXPERTS), 0.01),
        "w_gate_up": nrm(ks[24], (DEPTH, N_EXPERTS, D_MODEL, 2 * D_FF), D_MODEL ** -0.5),
        "b_gate_up": nrm(ks[25], (DEPTH, N_EXPERTS, 2 * D_FF), 0.01),
        "w_down": nrm(ks[26], (DEPTH, N_EXPERTS, D_FF, D_MODEL), D_FF ** -0.5 * DEEPNORM_BETA),
        "b_down": nrm(ks[27], (DEPTH, N_EXPERTS, D_MODEL), 0.01),
        "ln2_g": 1.0 + nrm(ks[28], (DEPTH, D_MODEL), 0.02),
        "ln2_b": nrm(ks[29], (DEPTH, D_MODEL), 0.02),
    }


def reference(x, meta_tokens, ln_in_g, ln_in_b, w_in, b_gate, conv_w, conv_b, dt_bias, a_log,
              d_skip, ssd_norm_w, w_ssd_out, lam_q1, lam_k1, lam_q2, lam_k2, subln_w, w_da_out,
              w_out, ln1_g, ln1_b, w_router, b_router, w_gate_up, b_gate_up, w_down, b_down,
              ln2_g, ln2_b):
    Bsz = x.shape[0]
    meta = jnp.broadcast_to(meta_tokens[None].astype(x.dtype), (Bsz, N_META, D_MODEL))
    h = layer_norm(jnp.concatenate([meta, x], axis=1), ln_in_g, ln_in_b)
    T = h.shape[1]
    pts = split_points(IN_SIZES)
    for l in range(DEPTH):
        lambda_init = 0.8 - 0.6 * math.exp(-0.3 * l)
        proj = h @ w_in[l]
        z, xbc, dt_raw, q, k, v, gate_logits = jnp.split(proj, pts, axis=-1)
        y_ssd = ssd_mixer(z, xbc, dt_raw, conv_w[l], conv_b[l], dt_bias[l], a_log[l],
                          d_skip[l], ssd_norm_w[l]) @ w_ssd_out[l]
        y_da = diff_attention(q.reshape(Bsz, T, DA_HEADS, 2, DA_HEAD_DIM),
                              k.reshape(Bsz, T, DA_HEADS, 2, DA_HEAD_DIM),
                              v.reshape(Bsz, T, DA_HEADS, DA_V_DIM),
                              lam_q1[l], lam_k1[l], lam_q2[l], lam_k2[l], subln_w[l],
                              lambda_init) @ w_da_out[l]
        gates = jax.nn.sigmoid((gate_logits + b_gate[l]).astype(jnp.float32)).astype(h.dtype)
        merged = gates[..., :D_MODEL] * y_ssd + gates[..., D_MODEL:] * y_da
        mix = (merged @ w_out[l]).astype(h.dtype)
        h = layer_norm(DEEPNORM_ALPHA * h + mix, ln1_g[l], ln1_b[l])
        ffn = moe_ffn(h, w_router[l], b_router[l], w_gate_up[l], b_gate_up[l], w_down[l], b_down[l])
        h = layer_norm(DEEPNORM_ALPHA * h + ffn, ln2_g[l], ln2_b[l])
    return h[:, N_META:]
```

```python
import numpy as np
import concourse.bass as bass
import concourse.mybir as mybir
from concourse.bass import IndirectOffsetOnAxis
from concourse.bass_utils import run_bass_kernel_spmd

F32 = mybir.dt.float32
BF16 = mybir.dt.bfloat16
I32 = mybir.dt.int32
U32 = mybir.dt.uint32
ALU = mybir.AluOpType
AF = mybir.ActivationFunctionType
AX = mybir.AxisListType

PE, ACT, DVE, POOL, SP = "pe", "act", "dve", "pool", "sp"
ENGS = (PE, ACT, DVE, POOL, SP)

D = 1024
SEQ = 4096
NMETA = 16
T = SEQ + NMETA
NTILE = 33
INCOLS = 8208
NEXP = 32
CAP = 768
NSLOT = NEXP * CAP
ALPHA = 2.0 ** 0.25
LN_EPS = 1e-5
RMS_EPS = 1e-6
LAMBDA_INIT = 0.2
C_Z, C_XBC, C_DT, C_Q, C_K, C_V, C_G = 0, 1024, 3072, 3088, 4112, 5136, 6160


def tcol(i):
    return (0, 16) if i == 0 else (16 + 128 * (i - 1), 128)


class Op:
    __slots__ = ("eng", "fn", "dma", "deps", "sig", "sem", "val", "prev", "name")


class Prog:
    def __init__(self, nc, n_sp=32, n_pool=16, n_act=40):
        self.nc = nc
        self.ops = []
        self.last_w = {}
        self.readers = {}
        self.n_dma = {SP: n_sp, POOL: n_pool, ACT: n_act}
        self.canon = {}

    def alias(self, *keys):
        base = self.canon.get(keys[0], keys[0])
        for k in keys[1:]:
            old = self.canon.get(k, k)
            if old == base:
                continue
            assert old not in self.last_w and old not in self.readers, old
            for kk, vv in list(self.canon.items()):
                if vv == old:
                    self.canon[kk] = base
            self.canon[k] = base

    def add(self, eng, fn, reads=(), writes=(), dma=False, name=""):
        op = Op()
        op.eng, op.fn, op.dma, op.name = eng, fn, dma, name
        op.sig = dma
        reads = list(dict.fromkeys(self.canon.get(k, k) for k in reads))
        writes = list(dict.fromkeys(self.canon.get(k, k) for k in writes))
        deps = []
        for k in reads:
            w = self.last_w.get(k)
            if w is not None:
                deps.append(w)
        for k in writes:
            w = self.last_w.get(k)
            if w is not None and (w.dma or dma or w.eng != eng):
                deps.append(w)
            for r in self.readers.get(k, ()):
                if r is not op and (r.dma or dma or r.eng != eng):
                    deps.append(r)
        seen = set()
        op.deps = []
        for d_ in deps:
            if id(d_) not in seen:
                seen.add(id(d_))
                op.deps.append(d_)
                d_.sig = True
        for k in writes:
            self.last_w[k] = op
            self.readers[k] = []
        for k in reads:
            lst = self.readers.setdefault(k, [])
            if not dma:
                lst[:] = [r for r in lst if r.dma or r.eng != eng]
            lst.append(op)
        self.ops.append(op)
        return op

    def barrier(self, fence_fn):
        allkeys = [k for k in (set(self.last_w) | set(self.readers)) if not k.startswith("z_")]
        self.add(DVE, fence_fn, writes=allkeys + ["fence"])
        for e_ in (PE, ACT, POOL, SP):
            self.add(e_, (lambda e: e.nop()), reads=["fence"])

    def emit(self, block, sems):
        cnt = {e: 0 for e in ENGS}
        dcount = {}
        dlast = {}
        for op in self.ops:
            if op.dma:
                n = self.n_dma[op.eng]
                i = dcount.get(op.eng, 0)
                dcount[op.eng] = i + 1
                s = sems[(op.eng, i % n)]
                op.sem = s
                op.prev = dlast.get(id(s), 0)
                op.val = op.prev + 16
                dlast[id(s)] = op.val
            elif op.sig:
                cnt[op.eng] += 1
                op.sem = sems[op.eng]
                op.val = cnt[op.eng]
        self.maxcnt = dict(cnt)
        per = {e: [o for o in self.ops if o.eng == e] for e in ENGS}

        def run(eng_name, eng):
            known = {}
            for op in per[eng_name]:
                need = {}
                for d_ in op.deps:
                    k = id(d_.sem)
                    if need.get(k, (None, 0))[1] < d_.val:
                        need[k] = (d_.sem, d_.val)
                if op.dma and op.prev > 0:
                    k = id(op.sem)
                    if need.get(k, (None, 0))[1] < op.prev:
                        need[k] = (op.sem, op.prev)
                for k, (s, v) in need.items():
                    if known.get(k, 0) < v:
                        eng.wait_ge(s, v)
                        known[k] = v
                ins = op.fn(eng)
                if op.dma:
                    ins.then_inc(op.sem, 16)
                elif op.sig:
                    ins.then_inc(op.sem, 1)
            if eng_name == SP:
                for k, v in dlast.items():
                    pass

        self._run = run
        self._dlast = dlast
        return per


class Arena:
    def __init__(self, ap, words):
        self.ap, self.words, self.off = ap, words, 0

    def reset(self, keep=0):
        self.off = keep

    def alloc(self, shape, dt=F32):
        n = 1
        for s_ in shape[1:]:
            n *= s_
        words = n if dt in (F32, I32, U32) else (n + 1) // 2
        words = (words + 7) // 8 * 8
        assert self.off + words <= self.words, (self.off, words, self.words)
        sl = self.ap[:, self.off:self.off + words]
        self.off += words
        if dt != F32:
            sl = sl.bitcast(dt)
        sl = sl[:, 0:n]
        if len(shape) == 3:
            sl = sl.rearrange("p (a b) -> p a b", a=shape[1])
        elif len(shape) == 4:
            sl = sl.rearrange("p (a b c) -> p a b c", a=shape[1], b=shape[2])
        return sl


H0T_WORDS = 8 * T // 2
ARENA_WORDS = 35000 + H0T_WORDS


def build_program(debug=None, stop_after=99):
    nc = bass.Bass("TRN2", target_bir_lowering=False)
    P = Prog(nc)

    def din(name, shape, dt=F32):
        return nc.dram_tensor(name, list(shape), dt, kind="ExternalInput").ap()

    x_d = din("x", [SEQ, D])
    meta_d = din("meta", [NMETA, D])
    consts_d = din("consts", [128, 5 * 128 + 72])
    lnin_g_d = din("ln_in_g", [1, D])
    lnin_b_d = din("ln_in_b", [1, D])
    w_in_d = din("w_in", [D, INCOLS])
    convw_d = din("convw_t", [128, 64])
    convb_d = din("convb_t", [128, 16])
    dtb_d = din("dt_bias", [1, 16])
    alog_d = din("a_log", [1, 16])
    dsk_d = din("d_skip", [1, 16])
    nw_d = din("ssd_norm_w", [1, D])
    lam_d = din("lam", [4, 64])
    subln_d = din("subln_w", [1, 128])
    attc_d = din("attc", [1, 1024])
    bgate_d = din("b_gate", [1, 2048])
    wso_d = din("w_ssd_out", [D, D])
    wdo_d = din("w_da_out", [D, D])
    wo_d = din("w_out", [D, D])
    ln1g_d = din("ln1_g", [1, D])
    ln1b_d = din("ln1_b", [1, D])
    wr_d = din("w_router", [D, NEXP])
    br_d = din("b_router", [1, NEXP])
    wgu_d = din("w_gate_up", [NEXP, D, 2 * D])
    bgu_d = din("bgu_t", [128, NEXP * 16])
    wd_d = din("w_down", [NEXP, D, D])
    bd_d = din("b_down", [NEXP, D])
    sel_d = din("sel", [NEXP, NEXP * 128])
    ln2g_d = din("ln2_g", [1, D])
    ln2b_d = din("ln2_b", [1, D])
    out_d = nc.dram_tensor("out", [SEQ, D], F32, kind="ExternalOutput").ap()
    dbg_d = dbgb_d = None
    if debug:
        dbg_d = nc.dram_tensor("dbg", [T, D], F32, kind="ExternalOutput").ap()
        dbgb_d = nc.dram_tensor("dbgb", [T, D], BF16, kind="ExternalOutput").ap()
    h0_scr = nc.dram_tensor("h0_scr", [T, D], F32).ap()
    g_scr = nc.dram_tensor("g_scr", [SEQ, D], BF16).ap()
    o_scr = nc.dram_tensor("o_scr", [SEQ, D], BF16).ap()
    sg_scr = nc.dram_tensor("sg_scr", [SEQ, 2 * D], BF16).ap()
    h1_scr = nc.dram_tensor("h1_scr", [SEQ, D], F32).ap()
    xs_scr = nc.dram_tensor("xs_scr", [NSLOT, D], BF16).ap()
    acc_scr = nc.dram_tensor("acc_scr", [SEQ + 128, D], F32).ap()
    meta_scr = nc.dram_tensor("meta_scr", [NSLOT, 2], F32).ap()

    import contextlib
    es = contextlib.ExitStack()
    with es:
        def sb(name, shape, dt=F32):
            return es.enter_context(nc.sbuf_tensor("sb_" + name, list(shape), dt))

        def ps(name, shape, dt=F32):
            return es.enter_context(nc.psum_tensor("ps_" + name, list(shape), dt))

        consts = sb("consts", [128, 5 * 128 + 72])
        ident = consts[:, 0:128]
        m_le = consts[:, 128:256]
        m_gt = consts[:, 256:384]
        m_lt = consts[:, 384:512]
        ones = consts[:, 512:640]
        ebase = consts[:, 640:672]
        emax = consts[:, 672:704]
        vcol = consts[:, 704:705]
        ident_bf = sb("ident_bf", [128, 128], BF16)
        small = sb("small", [128, 64])
        eps_t = small[:, 0:1]
        rmseps_t = small[:, 1:2]
        fence_t = small[:, 2:4]
        one_t = small[:, 4:5]
        mhalf_t = small[:, 5:6]
        arena_t = sb("arena", [128, ARENA_WORDS])
        AR = Arena(arena_t[:, :], ARENA_WORDS)
        h0T = AR.alloc([128, 8, T], BF16)
        assert AR.off == H0T_WORDS
        gate_all = sb("gate_all", [128, 32, 4])
        dest_all = sb("dest_all", [128, 32, 4], I32)
        psall = ps("psall", [128, 4096])
        psb = [psall[:, i * 512:(i + 1) * 512] for i in range(8)]
        PK = [f"ps{i}" for i in range(8)]

        def psbf(i):
            return psb[i].bitcast(BF16)

        P.add(SP, lambda e: e.dma_start(out=consts[:], in_=consts_d), writes=["consts"], dma=True)
        P.add(DVE, lambda e: e.tensor_copy(out=ident_bf[:], in_=ident), reads=["consts"], writes=["ident_bf"])
        P.add(DVE, lambda e: e.memset(eps_t, LN_EPS), writes=["eps"])
        P.add(DVE, lambda e: e.memset(rmseps_t, RMS_EPS), writes=["eps"])
        P.add(DVE, lambda e: e.memset(one_t, 1.0), writes=["eps"])
        P.add(DVE, lambda e: e.memset(mhalf_t, -0.5), writes=["eps"])

        def barrier():
            P.barrier(lambda e: e.memset(fence_t, 0.0))

        def bcast_load(dst, src_row, key):
            P.add(SP, lambda e: e.dma_start(out=dst, in_=src_row.partition_broadcast(128)), writes=[key], dma=True)

        def layer_norm_tile(xin, n, gb, bb, out_ap, keys_in, key_out, tag, stat, gbkeys, eng2=POOL, eng3=None, rstd_pool=False):
            eng3 = eng3 or eng2
            st6 = stat[:, 0:12]
            mv = stat[:, 12:14]
            rstd = stat[:, 14:15]
            skey = tag + "_stat"
            P.alias(*(list(keys_in) + [key_out + "_n", key_out + "_g", key_out]))
            P.alias(skey + "r0", skey + "r")
            P.add(DVE, lambda e: e.bn_stats(out=st6[:n, 0:6], in_=xin[:n, 0:512]), reads=keys_in, writes=[skey + "a"])
            P.add(DVE, lambda e: e.bn_stats(out=st6[:n, 6:12], in_=xin[:n, 512:1024]), reads=keys_in, writes=[skey + "b"])
            P.add(DVE, lambda e: e.bn_aggr(out=mv[:n], in_=st6[:n]), reads=[skey + "a", skey + "b"], writes=[skey + "mv"])
            if rstd_pool:
                P.add(POOL, lambda e: e.tensor_scalar(out=rstd[:n], in0=mv[:n, 1:2], scalar1=LN_EPS, scalar2=None, op0=ALU.add),
                      reads=[skey + "mv"], writes=[skey + "r0"])
                P.add(POOL, lambda e: e.tensor_tensor(out=rstd[:n], in0=rstd[:n], in1=mhalf_t[:n], op=ALU.pow),
                      reads=[skey + "r0", "eps"], writes=[skey + "r"])
            else:
                P.add(ACT, lambda e: e.activation(out=rstd[:n], in_=mv[:n, 1:2], func=AF.Ln, bias=eps_t[:n], scale=1.0),
                      reads=[skey + "mv", "eps"], writes=[skey + "r0"])
                P.add(ACT, lambda e: e.activation(out=rstd[:n], in_=rstd[:n], func=AF.Exp, scale=-0.5),
                      reads=[skey + "r0"], writes=[skey + "r"])
            P.add(DVE, lambda e: e.tensor_scalar(out=out_ap[:n], in0=xin[:n], scalar1=mv[:n, 0:1], scalar2=rstd[:n],
                                                 op0=ALU.subtract, op1=ALU.mult),
                  reads=keys_in + [skey + "mv", skey + "r"], writes=[key_out + "_n"])
            P.add(eng2, lambda e: e.tensor_tensor(out=out_ap[:n], in0=out_ap[:n], in1=gb[:n], op=ALU.mult),
                  reads=[key_out + "_n"] + gbkeys, writes=[key_out + "_g"])
            P.add(eng3, lambda e: e.tensor_tensor(out=out_ap[:n], in0=out_ap[:n], in1=bb[:n], op=ALU.add),
                  reads=[key_out + "_g"] + gbkeys, writes=[key_out])

        stg_ctr = [0]

        def load_cast(dst, src2d, ncols, key, stage=None, cast_eng=ACT):
            for c0 in range(0, ncols, 1024):
                c1 = min(ncols, c0 + 1024)
                P.add(POOL, (lambda e, c0=c0, c1=c1: e.dma_start(
                    out=dst[:, :, c0:c1], in_=src2d[:, c0:c1].rearrange("(k p) c -> p k c", p=128))), writes=[key], dma=True)
            return

        def load_cast_staged(dst, src2d, ncols, key, stage, cast_eng=ACT):
            sw = stage[0].shape[2]
            for c0 in range(0, ncols, sw):
                c1 = min(ncols, c0 + sw)
                w = c1 - c0
                b = stg_ctr[0] % len(stage)
                stg_ctr[0] += 1
                sk = f"stg{b}"
                P.add(SP, (lambda e, b=b, c0=c0, c1=c1, w=w: e.dma_start(
                    out=stage[b][:, :, 0:w], in_=src2d[:, c0:c1].rearrange("(k p) c -> p k c", p=128))),
                    writes=[sk], dma=True)
                if cast_eng == ACT:
                    fn = (lambda e, b=b, c0=c0, c1=c1, w=w: e.copy(out=dst[:, :, c0:c1], in_=stage[b][:, :, 0:w]))
                else:
                    fn = (lambda e, b=b, c0=c0, c1=c1, w=w: e.tensor_copy(out=dst[:, :, c0:c1], in_=stage[b][:, :, 0:w]))
                P.add(cast_eng, fn, reads=[sk], writes=[key])

        AR.reset(H0T_WORDS)
        Wssd = AR.alloc([128, 8, 3088], BF16)
        load_cast(Wssd, w_in_d[:, 0:3088], 3088, "Wssd")
        WSSD_END = AR.off
        g_bc = AR.alloc([128, D])
        b_bc = AR.alloc([128, D])
        bcast_load(g_bc, lnin_g_d, "gb0")
        bcast_load(b_bc, lnin_b_d, "gb0b")
        NB0 = 4
        xt = [AR.alloc([128, D]) for i in range(NB0)]
        hb = [AR.alloc([128, D], BF16) for i in range(NB0)]
        stat0 = [AR.alloc([128, 16]) for i in range(NB0)]
        for i in range(NTILE):
            c0, n = tcol(i)
            b = i % NB0
            xk, hk = f"xt{b}", f"hb{b}"
            src = meta_d if i == 0 else x_d[(i - 1) * 128:i * 128, :]
            P.add(SP, (lambda e, b=b, n=n, src=src: e.dma_start(out=xt[b][:n], in_=src)), writes=[xk], dma=True)
            layer_norm_tile(xt[b], n, g_bc, b_bc, xt[b], [xk], xk + "h", f"ln0{b}", stat0[b], ["gb0", "gb0b"], eng2=DVE, eng3=POOL)
            P.add(POOL, (lambda e, b=b, n=n, c0=c0: e.dma_start(out=h0_scr[c0:c0 + n, :], in_=xt[b][:n])),
                  reads=[xk + "h"], writes=[f"h0s{i}"], dma=True)
            if debug == "h0":
                P.add(SP, (lambda e, b=b, n=n, c0=c0: e.dma_start(out=dbg_d[c0:c0 + n, :], in_=xt[b][:n])),
                      reads=[xk + "h"], writes=["dbg"], dma=True)
            P.add(ACT, (lambda e, b=b, n=n: e.copy(out=hb[b][:n], in_=xt[b][:n])), reads=[xk + "h"], writes=[hk])
            pb = i % 2
            pst = psbf(pb)

            def tr(e, b=b, n=n, pst=pst):
                ins = None
                for kc in range(8):
                    ins = e.transpose(out=pst[:, kc * 128:kc * 128 + n], in_=hb[b][:n, kc * 128:(kc + 1) * 128],
                                      identity=ident_bf[:n, :n])
                return ins
            P.add(PE, tr, reads=[hk, "ident_bf"], writes=[PK[pb]])
            P.add(DVE, (lambda e, n=n, c0=c0, pst=pst: e.tensor_copy(
                out=h0T[:, :, c0:c0 + n], in_=pst.rearrange("p (k t) -> p k t", k=8)[:, :, 0:n])),
                reads=[PK[pb]], writes=[PK[pb], f"h0T{i}"])
        H0T_ALL = [f"h0T{i}" for i in range(NTILE)]
        barrier()

        if stop_after >= 1:
            AR.reset(WSSD_END)
            convw = AR.alloc([128, 64])
            convb = AR.alloc([128, 16])
            P.add(SP, lambda e: e.dma_start(out=convw, in_=convw_d), writes=["convw"], dma=True)
            P.add(SP, lambda e: e.dma_start(out=convb, in_=convb_d), writes=["convb"], dma=True)
            diagw = AR.alloc([128, 64, 128], BF16)
            for j in range(64):
                P.add(DVE, (lambda e, j=j: e.tensor_scalar(out=diagw[:, j, :], in0=ident, scalar1=convw[:, j:j + 1],
                                                           scalar2=None, op0=ALU.mult)),
                      reads=["consts", "convw"], writes=["diagw"])
            par = AR.alloc([128, 64])
            dtb_bc, alog_bc, dsk_bc, A_bc = par[:, 0:16], par[:, 16:32], par[:, 32:48], par[:, 48:64]
            bcast_load(dtb_bc, dtb_d, "par0")
            bcast_load(alog_bc, alog_d, "par1")
            bcast_load(dsk_bc, dsk_d, "par2")
            P.add(ACT, lambda e: e.activation(out=A_bc, in_=alog_bc, func=AF.Exp), reads=["par1"], writes=["parA0"])
            P.add(DVE, lambda e: e.tensor_scalar(out=A_bc, in0=A_bc, scalar1=-1.0, scalar2=None, op0=ALU.mult),
                  reads=["parA0"], writes=["parA"])
            nw_bc = AR.alloc([128, D])
            bcast_load(nw_bc, nw_d, "nw")
            state = AR.alloc([128, 16, 64])
            state_bf = AR.alloc([128, 16, 64], BF16)
            P.add(DVE, lambda e: e.memset(state, 0.0), writes=["state"])
            P.add(DVE, lambda e: e.memset(state_bf, 0.0), writes=["state_bf"])
            xp = [AR.alloc([128, 515], BF16) for _ in range(2)]
            halo = AR.alloc([128, 16, 3], BF16)
            P.add(DVE, lambda e: e.memset(halo, 0.0), writes=[f"halo{c}" for c in range(16)])
            xact = AR.alloc([128, 16, 512], BF16)
            x_tm = AR.alloc([128, 16, 64], BF16)
            B_tm = AR.alloc([128, 512], BF16)
            szs = [AR.alloc([128, D], BF16) for _ in range(2)]
            sm = AR.alloc([128, 192])
            dtraw, e1, dtt, a_t = sm[:, 0:16], sm[:, 16:32], sm[:, 32:48], sm[:, 48:64]
            cstot, expcs, dte = sm[:, 64:96], sm[:, 96:112], sm[:, 112:128]
            cdec, dtdte, ss, rstd4 = sm[:, 128:144], sm[:, 144:160], sm[:, 160:164], sm[:, 164:168]
            rhsA = AR.alloc([128, 8, 128])
            Eexp = AR.alloc([128, 16, 128], BF16)
            cbm = AR.alloc([128, 4, 128])
            WT = AR.alloc([128, 16, 128], BF16)
            xdt = AR.alloc([128, 16, 64], BF16)
            xdd = AR.alloc([128, 16, 64], BF16)
            xD = AR.alloc([128, 16, 64], BF16)
            t1s = [AR.alloc([128, D]) for _ in range(2)]
            tailq = []
            gout = AR.alloc([128, D], BF16)
            junk = AR.alloc([128, 256])

            for cp_ in range(2):
                P.alias(f"t1_{cp_}", f"t10_{cp_}", f"t11_{cp_}", f"t1b0_{cp_}", f"t1b1_{cp_}", f"gy_{cp_}")
            P.alias("rstd4a", "rstd4")
            P.alias("dte0", "dte")
            groups = [(0, 16, [0])] + [(16 + 512 * g, 512, [1 + 4 * g + j for j in range(4)]) for g in range(8)]
            for gi, (gc0, N, tiles) in enumerate(groups):
                hkeys = [f"h0T{t_}" for t_ in tiles]
                def emit_mm(c):
                    b = c % 2
                    xk = f"xp{b}"

                    def mm(e, c=c, b=b, gc0=gc0, N=N):
                        ins = None
                        for kc in range(8):
                            ins = e.matmul(psb[b][:, 0:N], lhsT=Wssd[:, kc, 1024 + c * 128:1024 + (c + 1) * 128],
                                           rhs=h0T[:, kc, gc0:gc0 + N], start=(kc == 0), stop=(kc == 7))
                        return ins
                    P.add(PE, mm, reads=["Wssd"] + hkeys, writes=[PK[b]])
                    P.add(ACT, (lambda e, b=b, N=N: e.copy(out=xp[b][:, 3:3 + N], in_=psb[b][:, 0:N])),
                          reads=[PK[b]], writes=[PK[b], xk])
                    P.add(POOL, (lambda e, b=b, c=c: e.tensor_copy(out=xp[b][:, 0:3], in_=halo[:, c, :])),
                          reads=[f"halo{c}"], writes=[xk + "h"])

                def emit_cv(c):
                    b = c % 2
                    xk = f"xp{b}"

                    def cv(e, c=c, b=b, N=N):
                        ins = None
                        for kk in range(4):
                            ins = e.matmul(psb[2 + b][:, 0:N], lhsT=diagw[:, kk * 16 + c, :], rhs=xp[b][:, kk:kk + N],
                                           start=(kk == 0), stop=(kk == 3))
                        return ins
                    P.add(PE, cv, reads=["diagw", xk, xk + "h"], writes=[PK[2 + b]])
                    P.add(POOL, (lambda e, b=b, c=c, N=N: e.tensor_copy(out=halo[:, c, :], in_=xp[b][:, N:N + 3])),
                          reads=[xk], writes=[f"halo{c}"])
                    P.add(ACT, (lambda e, c=c, b=b, N=N: e.activation(out=xact[:, c, 0:N], in_=psb[2 + b][:, 0:N], func=AF.Silu,
                                                                     bias=convb[:, c:c + 1], scale=1.0)),
                          reads=[PK[2 + b], "convb"], writes=[PK[2 + b], "xact"])
                emit_mm(0)
                for c in range(16):
                    if c + 1 < 16:
                        emit_mm(c + 1)
                    emit_cv(c)
                for j, ti in enumerate(tiles):
                    off = 0 if ti == 0 else 128 * j
                    tc0, n = tcol(ti)
                    real = ti > 0
                    cp = ti % 2
                    sz = szs[cp]
                    t1 = t1s[cp]
                    gy = t1
                    szk = f"sz{cp}"
                    def trx(e, off=off, n=n):
                        ins = None
                        pv = psbf(0)
                        for c in range(8):
                            ins = e.transpose(out=pv[:n, c * 128:(c + 1) * 128], in_=xact[:, c, off:off + n], identity=ident_bf[:, :])
                        return ins
                    P.add(PE, trx, reads=["xact", "ident_bf"], writes=[PK[0]])
                    P.add(ACT, (lambda e, n=n: e.copy(out=x_tm[:n].rearrange("p h d -> p (h d)"), in_=psbf(0)[:n, :])),
                          reads=[PK[0]], writes=[PK[0], "x_tm"])

                    def trb(e, off=off, n=n):
                        ins = None
                        pv = psbf(1)
                        for c in range(4):
                            ins = e.transpose(out=pv[:n, c * 128:(c + 1) * 128], in_=xact[:, 8 + c, off:off + n], identity=ident_bf[:, :])
                        return ins
                    P.add(PE, trb, reads=["xact", "ident_bf"], writes=[PK[1]])
                    P.add(DVE, (lambda e, n=n: e.tensor_copy(out=B_tm[:n], in_=psbf(1)[:n, 0:512])),
                          reads=[PK[1]], writes=[PK[1], "B_tm"])
                    def mdt(e, tc0=tc0, n=n):
                        ins = None
                        for kc in range(8):
                            ins = e.matmul(psb[1][:n, 256:272], lhsT=h0T[:, kc, tc0:tc0 + n], rhs=Wssd[:, kc, 3072:3088],
                                           start=(kc == 0), stop=(kc == 7))
                        return ins
                    P.add(PE, mdt, reads=["Wssd", f"h0T{ti}"], writes=[PK[1]])
                    P.add(DVE, (lambda e, n=n: e.tensor_tensor(out=dtraw[:n], in0=psb[1][:n, 256:272], in1=dtb_bc[:n], op=ALU.add)),
                          reads=[PK[1], "par0"], writes=[PK[1], "dtraw"])
                    P.add(ACT, (lambda e, n=n: e.activation(out=e1[:n], in_=dtraw[:n], func=AF.Exp)), reads=["dtraw"], writes=["e1"])
                    P.add(ACT, (lambda e, n=n: e.activation(out=dtt[:n], in_=e1[:n], func=AF.Ln, bias=one_t[:n], scale=1.0)),
                          reads=["e1", "eps"], writes=["dtt"])
                    P.add(DVE, (lambda e, n=n: e.tensor_tensor(out=a_t[:n], in0=dtt[:n], in1=A_bc[:n], op=ALU.mult)),
                          reads=["dtt", "parA"], writes=["a_t"])
                    def mcs(e, n=n):
                        e.matmul(psb[1][:n, 272:288], lhsT=m_le[:n, :n], rhs=a_t[:n, :], start=True, stop=True)
                        return e.matmul(psb[1][:, 288:304], lhsT=ones[:n, :], rhs=a_t[:n, :], start=True, stop=True)
                    P.add(PE, mcs, reads=["a_t", "consts"], writes=[PK[1]])
                    P.add(ACT, (lambda e: e.copy(out=cstot[:, :], in_=psb[1][:, 272:304])), reads=[PK[1]], writes=[PK[1], "cstot"])
                    P.add(ACT, (lambda e: e.activation(out=cdec[:, :], in_=cstot[:, 16:32], func=AF.Exp)), reads=["cstot"], writes=["cdec"])
                    P.add(DVE, (lambda e, n=n: e.tensor_tensor(out=dte[:n], in0=cstot[:n, 16:32], in1=cstot[:n, 0:16], op=ALU.subtract)),
                          reads=["cstot"], writes=["dte0"])
                    P.add(ACT, (lambda e, n=n: e.activation(out=dte[:n], in_=dte[:n], func=AF.Exp)), reads=["dte0"], writes=["dte"])
                    P.add(DVE, (lambda e, n=n: e.tensor_tensor(out=dtdte[:n], in0=dte[:n], in1=dtt[:n], op=ALU.mult)),
                          reads=["dte", "dtt"], writes=["dtdte"])
                    P.add(POOL, (lambda e, n=n: e.tensor_tensor(out=xdd[:n], in0=x_tm[:n], in1=dtdte[:n].unsqueeze(2).to_broadcast([n, 16, 64]), op=ALU.mult)),
                          reads=["x_tm", "dtdte"], writes=["xdd"])
                    if real:
                        P.add(ACT, (lambda e, n=n: e.activation(out=expcs[:n], in_=cstot[:n, 0:16], func=AF.Exp)), reads=["cstot"], writes=["expcs"])
                        P.add(DVE, (lambda e, n=n: e.tensor_tensor(out=xdt[:n], in0=x_tm[:n], in1=dtt[:n].unsqueeze(2).to_broadcast([n, 16, 64]), op=ALU.mult)),
                              reads=["x_tm", "dtt"], writes=["xdt"])
                        P.add(POOL, (lambda e, n=n: e.tensor_tensor(out=xD[:n], in0=x_tm[:n], in1=dsk_bc[:n].unsqueeze(2).to_broadcast([n, 16, 64]), op=ALU.mult)),
                              reads=["x_tm", "par2"], writes=["xD"])
                        for hf in range(2):
                            def mz(e, hf=hf, tc0=tc0, n=n):
                                ins = None
                                for kc in range(8):
                                    ins = e.matmul(psb[2 + hf][:n, :], lhsT=h0T[:, kc, tc0:tc0 + n], rhs=Wssd[:, kc, hf * 512:(hf + 1) * 512],
                                                   start=(kc == 0), stop=(kc == 7))
                                return ins
                            P.add(PE, mz, reads=["Wssd", f"h0T{ti}"], writes=[PK[2 + hf]])
                            P.add(ACT, (lambda e, hf=hf, n=n, sz=sz: e.activation(out=sz[:n, hf * 512:(hf + 1) * 512], in_=psb[2 + hf][:n, :], func=AF.Silu)),
                                  reads=[PK[2 + hf]], writes=[PK[2 + hf], szk])
                        for hh2 in range(2):
                            P.add(DVE, (lambda e, n=n, hh2=hh2: e.tensor_tensor(
                                out=rhsA[:n], in0=m_le[:n, :].unsqueeze(1).to_broadcast([n, 8, 128]),
                                in1=a_t[:n, 8 * hh2:8 * hh2 + 8].unsqueeze(2).to_broadcast([n, 8, 128]), op=ALU.mult)),
                                reads=["a_t", "consts"], writes=["rhsA"])
                            for q2 in range(2):
                                q = 2 * hh2 + q2
                                P.add(PE, (lambda e, q=q, q2=q2, n=n: e.matmul(psb[4 + q][:n, :], lhsT=m_gt[:n, :n],
                                                                              rhs=rhsA[:n, 4 * q2:4 * q2 + 4, :].rearrange("p h l -> p (h l)"),
                                                                              start=True, stop=True)),
                                      reads=["rhsA", "consts"], writes=[PK[4 + q]])
                                P.add(ACT, (lambda e, q=q, n=n: e.activation(out=Eexp[:n, 4 * q:4 * q + 4, :].rearrange("p h l -> p (h l)"),
                                                                            in_=psb[4 + q][:n, :], func=AF.Exp)),
                                      reads=[PK[4 + q]], writes=[PK[4 + q], "Eexp"])
                        def mcb(e, off=off, n=n):
                            ins = None
                            for g in range(4):
                                ins = e.matmul(psb[0][:n, g * 128:(g + 1) * 128], lhsT=xact[:, 8 + g, off:off + n], rhs=xact[:, 12 + g, off:off + n],
                                               start=True, stop=True)
                            return ins
                        P.add(PE, mcb, reads=["xact"], writes=[PK[0]])
                        P.add(DVE, (lambda e, n=n: e.tensor_tensor(out=cbm[:n], in0=psb[0][:n, :].rearrange("p (g l) -> p g l", g=4),
                                                                  in1=m_le[:n, :].unsqueeze(1).to_broadcast([n, 4, 128]), op=ALU.mult)),
                              reads=[PK[0], "consts"], writes=[PK[0], "cbm"])
                        P.add(DVE, (lambda e, n=n: e.tensor_tensor(out=WT[:n].rearrange("p (g r) l -> p g r l", g=4),
                                                                  in0=Eexp[:n].rearrange("p (g r) l -> p g r l", g=4),
                                                                  in1=cbm[:n].unsqueeze(2).to_broadcast([n, 4, 4, 128]), op=ALU.mult)),
                              reads=["Eexp", "cbm"], writes=["WT"])
                        for hf in range(2):
                            def myd(e, hf=hf, n=n):
                                ins = e.matmul(psb[4 + hf][:n, :], lhsT=ident_bf[:n, :n], rhs=xD[:n, 8 * hf:8 * hf + 8, :].rearrange("p h d -> p (h d)"),
                                               start=True, stop=False, skip_group_check=True)
                                for hh in range(8):
                                    h_ = 8 * hf + hh
                                    ins = e.matmul(psb[4 + hf][:n, hh * 64:(hh + 1) * 64], lhsT=WT[:n, h_, :n], rhs=xdt[:n, h_, :],
                                                   start=False, stop=(hh == 7), skip_group_check=True)
                                return ins
                            P.add(PE, myd, reads=["ident_bf", "xD", "WT", "xdt"], writes=[PK[4 + hf]])

                            def myo(e, hf=hf, off=off, n=n):
                                ins = None
                                for gg in range(2):
                                    g = 2 * hf + gg
                                    ins = e.matmul(psb[6 + hf][:n, gg * 256:(gg + 1) * 256], lhsT=xact[:, 12 + g, off:off + n],
                                                   rhs=state_bf[:, 4 * g:4 * g + 4, :].rearrange("p h d -> p (h d)"), start=True, stop=True)
                                return ins
                            P.add(PE, myo, reads=["xact", "state_bf"], writes=[PK[6 + hf]])
                    for hf in range(2):
                        def mst(e, hf=hf, n=n):
                            ins = None
                            for gg in range(2):
                                g = 2 * hf + gg
                                ins = e.matmul(psb[2 + hf][:, gg * 256:(gg + 1) * 256], lhsT=B_tm[:n, g * 128:(g + 1) * 128],
                                               rhs=xdd[:n, 4 * g:4 * g + 4, :].rearrange("p h d -> p (h d)"), start=True, stop=True)
                            return ins
                        P.add(PE, mst, reads=["B_tm", "xdd"], writes=[PK[2 + hf]])
                    P.add(DVE, (lambda e: e.tensor_tensor(out=state, in0=state, in1=cdec[:, :].unsqueeze(2).to_broadcast([128, 16, 64]), op=ALU.mult)),
                          reads=["state", "cdec"], writes=["state"])
                    for hf in range(2):
                        P.add(DVE, (lambda e, hf=hf: e.tensor_tensor(out=state[:, 8 * hf:8 * hf + 8, :].rearrange("p h d -> p (h d)"),
                                                                    in0=state[:, 8 * hf:8 * hf + 8, :].rearrange("p h d -> p (h d)"),
                                                                    in1=psb[2 + hf][:, :], op=ALU.add)),
                              reads=["state", PK[2 + hf]], writes=[PK[2 + hf], "state"])
                    P.add(ACT, (lambda e: e.copy(out=state_bf, in_=state)), reads=["state"], writes=["state_bf"])
                    if real:
                        for hf in range(2):
                            P.add(DVE, (lambda e, hf=hf, n=n, t1=t1: e.tensor_tensor(
                                out=t1[:n, hf * 512:(hf + 1) * 512].rearrange("p (h d) -> p h d", h=8),
                                in0=psb[6 + hf][:n, :].rearrange("p (h d) -> p h d", h=8),
                                in1=expcs[:n, 8 * hf:8 * hf + 8].unsqueeze(2).to_broadcast([n, 8, 64]), op=ALU.mult)),
                                reads=[PK[6 + hf], "expcs"], writes=[PK[6 + hf], f"t1{hf}_{cp}"])
                            P.add(DVE, (lambda e, hf=hf, n=n, t1=t1: e.tensor_tensor(out=t1[:n, hf * 512:(hf + 1) * 512], in0=t1[:n, hf * 512:(hf + 1) * 512],
                                                                             in1=psb[4 + hf][:n, :], op=ALU.add)),
                                  reads=[PK[4 + hf], f"t1{hf}_{cp}"], writes=[PK[4 + hf], f"t1b{hf}_{cp}"])
                        def tail(n=n, ti=ti, cp=cp, sz=sz, t1=t1, gy=gy, szk=szk):
                            gk = f"gy_{cp}"
                            P.add(DVE, (lambda e: e.tensor_tensor(out=gy[:n], in0=t1[:n], in1=sz[:n], op=ALU.mult)),
                                  reads=[f"t1b0_{cp}", f"t1b1_{cp}", szk], writes=[gk])
                            for g in range(4):
                                P.add(ACT, (lambda e, g=g: e.activation(out=junk[:n], in_=gy[:n, g * 256:(g + 1) * 256], func=AF.Square,
                                                                       accum_out=ss[:n, g:g + 1])),
                                      reads=[gk], writes=["junk", f"ss{g}"])
                            P.add(ACT, (lambda e: e.activation(out=rstd4[:n], in_=ss[:n], func=AF.Ln, bias=rmseps_t[:n], scale=1.0 / 256)),
                                  reads=[f"ss{g}" for g in range(4)] + ["eps"], writes=["rstd4a"])
                            P.add(ACT, (lambda e: e.activation(out=rstd4[:n], in_=rstd4[:n], func=AF.Exp, scale=-0.5)),
                                  reads=["rstd4a"], writes=["rstd4"])
                            for g in range(4):
                                P.add(DVE, (lambda e, g=g: e.scalar_tensor_tensor(out=gout[:n, g * 256:(g + 1) * 256], in0=gy[:n, g * 256:(g + 1) * 256],
                                                                                 scalar=rstd4[:n, g:g + 1], in1=nw_bc[:n, g * 256:(g + 1) * 256],
                                                                                 op0=ALU.mult, op1=ALU.mult)),
                                      reads=[gk, "rstd4", "nw"], writes=["gout"])
                            r0 = (ti - 1) * 128
                            P.add(SP, (lambda e: e.dma_start(out=g_scr[r0:r0 + 128, :], in_=gout[:, :])), reads=["gout"], writes=[f"gscr{ti}"], dma=True)
                            if debug == "gssd":
                                P.add(SP, (lambda e: e.dma_start(out=dbgb_d[16 + r0:16 + r0 + 128, :], in_=gout[:, :])), reads=["gout"], writes=["dbg"], dma=True)
                        while tailq:
                            tailq.pop(0)()
                        tailq.append(tail)
            while tailq:
                tailq.pop(0)()
            barrier()


        if stop_after >= 2:
            AR.reset(H0T_WORDS)
            stage = None
            Wh = AR.alloc([128, 8, 384], BF16)
            QT = AR.alloc([128, T], BF16)
            KT = AR.alloc([128, T], BF16)
            Vaug = AR.alloc([128, NTILE, 130], BF16)
            PT = [AR.alloc([128, 2, 512], BF16) for _ in range(3)]
            attc_f = AR.alloc([128, 1024])
            attc = AR.alloc([128, 1024], BF16)
            P.add(SP, lambda e: e.dma_start(out=attc_f[0:1, :], in_=attc_d), writes=["attc_f"], dma=True)
            P.add(DVE, lambda e: e.tensor_copy(out=attc[0:1, :], in_=attc_f[0:1, :]), reads=["attc_f"], writes=["attc"])
            maskk = attc[0:1, 0:128]
            onesr = attc[0:1, 128:640]
            zeror = attc[0:1, 640:1024]
            lamv = AR.alloc([128, 4, 64])
            for i4 in range(4):
                bcast_load(lamv[:, i4, :], lam_d[i4:i4 + 1, :], "lamv")
            lsm = AR.alloc([128, 16])
            ljunk = AR.alloc([128, 64])
            for i2 in range(2):
                P.add(DVE, (lambda e, i2=i2: e.scalar_tensor_tensor(out=ljunk, in0=lamv[:, 2 * i2, :], scalar=1.0, in1=lamv[:, 2 * i2 + 1, :],
                                                                   op0=ALU.mult, op1=ALU.mult, accum_out=lsm[:, i2:i2 + 1])),
                      reads=["lamv"], writes=["ljunk", f"lsm{i2}"])
            P.add(ACT, lambda e: e.activation(out=lsm[:, 2:4], in_=lsm[:, 0:2], func=AF.Exp), reads=["lsm0", "lsm1"], writes=["lsme"])
            P.add(DVE, lambda e: e.tensor_tensor(out=lsm[:, 4:5], in0=lsm[:, 3:4], in1=lsm[:, 2:3], op=ALU.subtract), reads=["lsme"], writes=["nl0"])
            P.add(DVE, lambda e: e.tensor_scalar(out=lsm[:, 5:6], in0=lsm[:, 4:5], scalar1=-LAMBDA_INIT, scalar2=None, op0=ALU.add), reads=["nl0"], writes=["neglam"])
            neglam = lsm[:, 5:6]
            subw = AR.alloc([128, 128])
            bcast_load(subw, subln_d, "subw0")
            P.add(DVE, lambda e: e.tensor_scalar(out=subw, in0=subw, scalar1=1.0 - LAMBDA_INIT, scalar2=None, op0=ALU.mult), reads=["subw0"], writes=["subw"])
            accsb = [AR.alloc([128, 8, 130]) for _ in range(2)]
            fin = [AR.alloc([128, 16]) for _ in range(2)]
            oq = [AR.alloc([128, 4, 128]) for _ in range(2)]
            osq = [AR.alloc([128, 4, 128]) for _ in range(2)]
            osb = [AR.alloc([128, 4, 128], BF16) for _ in range(2)]
            pending = []
            gpend = []
            zt = AR.alloc([128, 4096], BF16)
            ztf = zt.bitcast(F32)
            P.add(POOL, lambda e: e.memset(zt, 0.0), writes=["z_zt"])
            xs_flat = xs_scr.rearrange("s d -> (s d)").rearrange("(p f) -> p f", p=128)
            acc_flat = acc_scr.rearrange("s d -> (s d)").rearrange("(p f) -> p f", p=128)
            meta_flat = meta_scr.rearrange("s d -> (s d)").rearrange("(p f) -> p f", p=128)
            nacc = (SEQ + 128) * D // 128
            zfills = []
            for j in range(NSLOT * D // 128 // 4096):
                zfills.append((lambda j=j: P.add(POOL, (lambda e: e.dma_start(out=xs_flat[:, j * 4096:(j + 1) * 4096], in_=zt)), reads=["z_zt"], writes=[f"zf_xs{j}"], dma=True)))
            for j0 in range(0, nacc, 2048):
                w_ = min(2048, nacc - j0)
                zfills.append((lambda j0=j0, w_=w_: P.add(POOL, (lambda e: e.dma_start(out=acc_flat[:, j0:j0 + w_], in_=ztf[:, 0:w_])), reads=["z_zt"], writes=[f"zf_acc{j0}"], dma=True)))
            zfills.append((lambda: P.add(POOL, lambda e: e.dma_start(out=meta_flat, in_=ztf[:, 0:NSLOT * 2 // 128]), reads=["z_zt"], writes=["zf_meta"], dma=True)))
            P.add(DVE, lambda e: e.memset(Vaug[:, :, 128:130], 1.0), writes=["Vaug1"])
            Wg = AR.alloc([128, 8, 2048], BF16)
            load_cast(Wg, w_in_d[:, C_G:C_G + 2048], 2048, "Wg", stage)
            bgate = AR.alloc([128, 2048], BF16)
            P.add(POOL, lambda e: e.dma_start(out=bgate[0:1, :], in_=bgate_d), writes=["bgate"], dma=True)
            sgt = [AR.alloc([128, 2048], BF16) for _ in range(2)]
            sge = [AR.alloc([128, 512]) for _ in range(2)]
            blocks = [(0, 16)] + [(16 + 512 * g, 512) for g in range(8)]
            it = 0
            for h in range(8):
                for pi, cbase in enumerate((C_Q, C_K, C_V)):
                    load_cast(Wh[:, :, pi * 128:(pi + 1) * 128], w_in_d[:, cbase + h * 128:cbase + (h + 1) * 128], 128, "Wh", stage)
                for _z in range((len(zfills) + 7 - h) // (8 - h) if h < 7 else len(zfills)):
                    if zfills:
                        zfills.pop(0)()
                for which, dst, scale in ((0, QT, 0.125), (1, KT, 1.0)):
                    for (bc0, N) in blocks:
                        if which == 0 and bc0 == 0:
                            continue
                        def mp(e, which=which, bc0=bc0, N=N):
                            ins = None
                            for kc in range(8):
                                ins = e.matmul(psb[7][:, 0:N], lhsT=Wh[:, kc, which * 128:(which + 1) * 128], rhs=h0T[:, kc, bc0:bc0 + N],
                                               start=(kc == 0), stop=(kc == 7))
                            return ins
                        P.add(PE, mp, reads=["Wh"] + H0T_ALL, writes=[PK[7]])
                        P.add(DVE, (lambda e, dst=dst, bc0=bc0, N=N, scale=scale: e.tensor_scalar(
                            out=dst[:, bc0:bc0 + N], in0=psb[7][:, 0:N], scalar1=scale, scalar2=None, op0=ALU.mult)),
                            reads=[PK[7]], writes=[PK[7], "QT" if which == 0 else "KT"])
                for t0 in range(0, NTILE, 4):
                    tl = list(range(t0, min(NTILE, t0 + 4)))
                    def mv(e, tl=tl):
                        ins = None
                        for si, ti in enumerate(tl):
                            tc0, n = tcol(ti)
                            for kc in range(8):
                                ins = e.matmul(psb[7][:n, si * 128:(si + 1) * 128], lhsT=h0T[:, kc, tc0:tc0 + n], rhs=Wh[:, kc, 256:384],
                                               start=(kc == 0), stop=(kc == 7))
                        return ins
                    P.add(PE, mv, reads=["Wh"] + H0T_ALL, writes=[PK[7]])
                    nt = len(tl)
                    if t0 == 0:
                        P.add(DVE, (lambda e: e.tensor_copy(out=Vaug[:16, 0, 0:128], in_=psb[7][:16, 0:128])), reads=[PK[7]], writes=[PK[7], "Vaug"])
                        P.add(DVE, (lambda e, nt=nt: e.tensor_copy(out=Vaug[:, 1:nt, 0:128], in_=psb[7][:, 128:nt * 128].rearrange("p (t c) -> p t c", c=128))),
                              reads=[PK[7]], writes=[PK[7], "Vaug"])
                    else:
                        P.add(DVE, (lambda e, t0=t0, nt=nt: e.tensor_copy(out=Vaug[:, t0:t0 + nt, 0:128], in_=psb[7][:, 0:nt * 128].rearrange("p (t c) -> p t c", c=128))),
                              reads=[PK[7]], writes=[PK[7], "Vaug"])
                for ti in range(4 * h + 1, 4 * h + 5):
                    tc0, n = tcol(ti)
                    sb2 = ti % 2
                    for cc in range(4):
                        def gunit(tc0=tc0, cc=cc, sb2=sb2, ti=ti):
                            def mg(e):
                                for kc in range(8):
                                    e.matmul(psb[7][:, :], lhsT=h0T[:, kc, tc0:tc0 + 128], rhs=Wg[:, kc, cc * 512:(cc + 1) * 512],
                                             start=(kc == 0), stop=False)
                                return e.matmul(psb[7][:, :], lhsT=onesr[0:1, 0:128], rhs=bgate[0:1, cc * 512:(cc + 1) * 512], start=False, stop=True)
                            P.add(PE, mg, reads=["Wg", "bgate", "attc", f"h0T{ti}"], writes=[PK[7]])
                            eb = cc % 2
                            P.add(ACT, (lambda e: e.activation(out=sge[eb], in_=psb[7][:, :], func=AF.Exp, scale=-1.0)),
                                  reads=[PK[7]], writes=[PK[7], f"sge{eb}"])
                            P.add(DVE, (lambda e: e.tensor_scalar(out=sge[eb], in0=sge[eb], scalar1=1.0, scalar2=None, op0=ALU.add)),
                                  reads=[f"sge{eb}"], writes=[f"sge{eb}"])
                            P.add(DVE, (lambda e: e.reciprocal(out=sgt[sb2][:, cc * 512:(cc + 1) * 512], in_=sge[eb])),
                                  reads=[f"sge{eb}"], writes=[f"sgt{sb2}"])
                            if cc == 3:
                                P.add(SP, (lambda e: e.dma_start(out=sg_scr[(ti - 1) * 128:ti * 128, :], in_=sgt[sb2])),
                                      reads=[f"sgt{sb2}"], writes=[f"sgscr{ti}"], dma=True)
                        gpend.append(gunit)
                for G in range(8):
                    qc0 = 16 + 512 * G
                    gidx = h * 8 + G
                    ab = gidx % 2

                    def acc(qi, j):
                        a_ = qi * 2 + j
                        return psb[4 + a_ // 3][:, (a_ % 3) * 130:(a_ % 3) * 130 + 130]
                    klist = [("m", 0)] + [("r", kt) for kt in range(4 * G + 4)]
                    infos = []
                    for (kind, kt) in klist:
                        sb_ = 2 * (it % 2)
                        pts = it % 3
                        it += 1
                        if kind == "m":
                            kc0, kn, qi0, vt = 0, 16, 0, 0
                        else:
                            kc0, kn, qi0, vt = 16 + 128 * kt, 128, max(0, kt - 4 * G), 1 + kt
                        N = 128 * (4 - qi0)
                        q0 = qc0 + 128 * qi0
                        diag = (kind == "r" and kt >= 4 * G)
                        infos.append((sb_, pts, kc0, kn, qi0, vt, N, q0, diag))

                    def emit_scores(info):
                        sb_, pts, kc0, kn, qi0, vt, N, q0, diag = info

                        def ms(e):
                            ins = None
                            for j in range(2):
                                ins = e.matmul(psb[sb_ + j][:kn, 0:N], lhsT=KT[64 * j:64 * j + 64, kc0:kc0 + kn], rhs=QT[64 * j:64 * j + 64, q0:q0 + N],
                                               start=True, stop=True, skip_group_check=True)
                            if diag:
                                for j in range(2):
                                    ins = e.matmul(psb[sb_ + j][:, 0:64], lhsT=maskk, rhs=onesr[0:1, 0:64], start=False, stop=True, skip_group_check=True)
                            return ins
                        P.add(PE, ms, reads=["QT", "KT", "attc"], writes=[PK[sb_], PK[sb_ + 1]])
                        P.add(ACT, (lambda e: e.activation(
                            out=PT[pts][:kn, :, 0:N], in_=psall[:kn, sb_ * 512:(sb_ + 2) * 512].rearrange("p (j n) -> p j n", j=2)[:, :, 0:N], func=AF.Exp)),
                            reads=[PK[sb_], PK[sb_ + 1]], writes=[PK[sb_], PK[sb_ + 1], f"PT{pts}"])

                    def emit_pv(info):
                        sb_, pts, kc0, kn, qi0, vt, N, q0, diag = info

                        def mpv(e):
                            ins = None
                            for qi in range(qi0, 4):
                                for j in range(2):
                                    ins = e.matmul(acc(qi, j), lhsT=PT[pts][:kn, j, (qi - qi0) * 128:(qi - qi0 + 1) * 128], rhs=Vaug[:kn, vt, :],
                                                   start=False, stop=True, skip_group_check=True)
                            return ins
                        P.add(PE, mpv, reads=[f"PT{pts}", "Vaug", "Vaug1"], writes=[PK[4], PK[5], PK[6]])

                    emit_scores(infos[0])
                    def zi(e):
                        ins = None
                        for b_ in (4, 5, 6):
                            ins = e.matmul(psb[b_][:, :], lhsT=zeror[0:1, 0:128], rhs=attc[0:1, 512:1024], start=True, stop=True, skip_group_check=True)
                        return ins
                    P.add(PE, zi, reads=["attc"], writes=[PK[4], PK[5], PK[6]])
                    for ii in range(len(infos)):
                        if ii + 1 < len(infos):
                            emit_scores(infos[ii + 1])
                        emit_pv(infos[ii])
                        if pending:
                            pending.pop(0)()
                        if gpend and ii % 2 == 1:
                            gpend.pop(0)()
                    acs = accsb[ab]
                    for b_ in range(3):
                        na = 3 if b_ < 2 else 2
                        P.add(DVE, (lambda e, b_=b_, na=na, acs=acs: e.tensor_copy(
                            out=acs[:, 3 * b_:3 * b_ + na, :].rearrange("p a c -> p (a c)"), in_=psb[4 + b_][:, 0:na * 130])),
                            reads=[PK[4 + b_]], writes=[PK[4 + b_], f"accsb{ab}"])

                    def fin1(ab=ab, acs=acs):
                        fk = f"fin{ab}"
                        F = fin[ab]
                        P.add(DVE, (lambda e: e.reciprocal(out=F[:, 0:8], in_=acs[:, :, 128])), reads=[f"accsb{ab}"], writes=[fk])
                        P.add(DVE, (lambda e: e.tensor_scalar(out=F[:, 0:8].rearrange("p (q j) -> p q j", j=2)[:, :, 1], in0=F[:, 0:8].rearrange("p (q j) -> p q j", j=2)[:, :, 1],
                                                              scalar1=neglam, scalar2=None, op0=ALU.mult)), reads=[fk, "neglam"], writes=[fk])
                        P.add(DVE, (lambda e: e.tensor_tensor(out=acs[:, :, 0:128], in0=acs[:, :, 0:128], in1=F[:, 0:8].unsqueeze(2).to_broadcast([128, 8, 128]), op=ALU.mult)),
                              reads=[f"accsb{ab}", fk], writes=[f"accsb{ab}"])
                        a4 = acs.rearrange("p (q j) c -> p q j c", j=2)
                        P.add(DVE, (lambda e: e.tensor_tensor(out=oq[ab], in0=a4[:, :, 0, 0:128], in1=a4[:, :, 1, 0:128], op=ALU.add)),
                              reads=[f"accsb{ab}"], writes=[f"oq{ab}"])
                        P.add(DVE, (lambda e: e.tensor_tensor(out=osq[ab], in0=oq[ab], in1=oq[ab], op=ALU.mult)), reads=[f"oq{ab}"], writes=[f"osq{ab}"])
                        P.add(DVE, (lambda e: e.tensor_reduce(out=F[:, 8:12], in_=osq[ab], axis=AX.X, op=ALU.add)), reads=[f"osq{ab}"], writes=[fk])

                    def fin2(ab=ab):
                        fk = f"fin{ab}"
                        F = fin[ab]
                        P.add(ACT, (lambda e: e.activation(out=F[:, 12:16], in_=F[:, 8:12], func=AF.Ln, bias=rmseps_t, scale=1.0 / 128)), reads=[fk, "eps"], writes=[fk])
                        P.add(ACT, (lambda e: e.activation(out=F[:, 12:16], in_=F[:, 12:16], func=AF.Exp, scale=-0.5)), reads=[fk], writes=[fk])

                    def fin3(ab=ab, G=G, h=h):
                        fk = f"fin{ab}"
                        F = fin[ab]
                        P.add(DVE, (lambda e: e.tensor_tensor(out=oq[ab], in0=oq[ab], in1=F[:, 12:16].unsqueeze(2).to_broadcast([128, 4, 128]), op=ALU.mult)),
                              reads=[f"oq{ab}", fk], writes=[f"oq{ab}"])
                        P.add(DVE, (lambda e: e.tensor_tensor(out=osb[ab], in0=oq[ab], in1=subw.unsqueeze(1).to_broadcast([128, 4, 128]), op=ALU.mult)),
                              reads=[f"oq{ab}", "subw"], writes=[f"osb{ab}"])
                        r0 = 512 * G
                        P.add(SP, (lambda e: e.dma_start(
                            out=o_scr[r0:r0 + 512, h * 128:(h + 1) * 128].rearrange("(q p) c -> p q c", p=128), in_=osb[ab])),
                            reads=[f"osb{ab}"], writes=[f"oscr{G}"], dma=True)
                        if debug == "oda":
                            P.add(SP, (lambda e: e.dma_start(
                                out=dbgb_d[16 + r0:16 + r0 + 512, h * 128:(h + 1) * 128].rearrange("(q p) c -> p q c", p=128), in_=osb[ab])),
                                reads=[f"osb{ab}"], writes=["dbg"], dma=True)
                    pending.extend([fin1, fin2, fin3])
            while pending:
                pending.pop(0)()
            while gpend:
                gpend.pop(0)()
            barrier()

        if stop_after >= 3:
            AR.reset(0)
            stage = None
            Wso = AR.alloc([128, 8, D], BF16)
            Wdo = AR.alloc([128, 8, D], BF16)
            Wo = AR.alloc([128, 8, D], BF16)
            load_cast(Wso, wso_d, D, "Wso", stage)
            load_cast(Wdo, wdo_d, D, "Wdo", stage)
            load_cast(Wo, wo_d, D, "Wo", stage)
            l1g = AR.alloc([128, D])
            l1b = AR.alloc([128, D])
            bcast_load(l1g, ln1g_d, "l1g")
            bcast_load(l1b, ln1b_d, "l1b")
            wr = AR.alloc([128, 8, NEXP])
            P.add(SP, lambda e: e.dma_start(out=wr, in_=wr_d.rearrange("(k p) c -> p k c", p=128)), writes=["wr"], dma=True)
            brt = AR.alloc([128, NEXP])
            P.add(SP, lambda e: e.dma_start(out=brt[0:1, :], in_=br_d), writes=["brt"], dma=True)
            cnt_eb = AR.alloc([128, NEXP])
            P.add(DVE, lambda e: e.tensor_copy(out=cnt_eb, in_=ebase), reads=["consts"], writes=["cnt_eb"])
            NB3 = 3
            NL3 = 4
            gt = [AR.alloc([128, D], BF16) for _ in range(NL3)]
            ot = [AR.alloc([128, D], BF16) for _ in range(NL3)]
            sgl = [AR.alloc([128, 2 * D], BF16) for _ in range(NL3)]
            rr = [AR.alloc([128, D]) for _ in range(NL3)]
            gT = [AR.alloc([128, 8, 128], BF16) for _ in range(NB3)]
            oT = [AR.alloc([128, 8, 128], BF16) for _ in range(NB3)]
            mT = [AR.alloc([128, 8, 128], BF16) for _ in range(NB3)]
            m1 = [AR.alloc([128, D]) for _ in range(NB3)]
            mg_ = [AR.alloc([128, D], BF16) for _ in range(NB3)]
            h1bf = [AR.alloc([128, D], BF16) for _ in range(NL3)]
            h1T = [AR.alloc([128, 4, 128]) for _ in range(2)]
            stat3 = [AR.alloc([128, 16]) for _ in range(NL3)]
            rt = [AR.alloc([128, 160]) for _ in range(NL3)]
            meta4 = [AR.alloc([128, 4, 2]) for _ in range(NL3)]
            for b_ in range(NL3):
                x_ = f"_{b_}"
                P.alias("mgbuf" + x_, "mgA0" + x_, "mgA1" + x_, "mg0" + x_, "mg1" + x_)
                P.alias("m1buf" + x_, "m10" + x_, "m11" + x_)
                x_ = f"_R{b_}"
                P.alias("rr" + x_, "rr0" + x_, "rr1" + x_)
                P.alias("rt" + x_ + "sb0", "rt" + x_ + "sb")
                P.alias("rt" + x_ + "gsum", "rt" + x_ + "grs")

            def tr_to(src, dst, skey, dkey, bank, eng_copy):
                def trf(e, src=src, bank=bank):
                    ins = None
                    pv = psbf(bank)
                    for c in range(8):
                        ins = e.transpose(out=pv[:, c * 128:(c + 1) * 128], in_=src[:, c * 128:(c + 1) * 128], identity=ident_bf[:, :])
                    return ins
                P.add(PE, trf, reads=(skey if isinstance(skey, list) else [skey]) + ["ident_bf"], writes=[PK[bank]])
                if eng_copy == ACT:
                    P.add(ACT, (lambda e, dst=dst, bank=bank: e.copy(out=dst.rearrange("p k t -> p (k t)"), in_=psbf(bank))),
                          reads=[PK[bank]], writes=[PK[bank], dkey])
                else:
                    P.add(DVE, (lambda e, dst=dst, bank=bank: e.tensor_copy(out=dst.rearrange("p k t -> p (k t)"), in_=psbf(bank))),
                          reads=[PK[bank]], writes=[PK[bank], dkey])

            def lin(actT, W, akey, wkey, banks):
                for hf in range(2):
                    def ml(e, actT=actT, W=W, hf=hf, bank=banks[hf]):
                        ins = None
                        for kc in range(8):
                            ins = e.matmul(psb[bank][:, :], lhsT=actT[:, kc, :], rhs=W[:, kc, hf * 512:(hf + 1) * 512], start=(kc == 0), stop=(kc == 7))
                        return ins
                    P.add(PE, ml, reads=[akey, wkey], writes=[PK[banks[hf]]])

            def loads3(i):
                lb = i % NL3
                lx = f"_L{lb}"
                ti = i + 1
                r0 = 128 * i
                P.add(SP, (lambda e: e.dma_start(out=gt[lb], in_=g_scr[r0:r0 + 128, :])), reads=[f"gscr{ti}"], writes=["gt" + lx], dma=True)
                P.add(SP, (lambda e: e.dma_start(out=ot[lb], in_=o_scr[r0:r0 + 128, :])), reads=[f"oscr{i // 4}"], writes=["ot" + lx], dma=True)
                P.add(SP, (lambda e: e.dma_start(out=sgl[lb], in_=sg_scr[r0:r0 + 128, :])), reads=[f"sgscr{ti}"], writes=["sgl" + lx], dma=True)

            def front3(i):
                b = i % NB3
                lb = i % NL3
                sfx = f"_{b}"
                lx = f"_L{lb}"
                ti = i + 1
                tc0 = 16 + 128 * i
                P.add(SP, (lambda e: e.dma_start(out=rr[lb], in_=h0_scr[tc0:tc0 + 128, :])), reads=[f"h0s{ti}"], writes=["rr" + f"_R{lb}"], dma=True)
                tr_to(gt[lb], gT[b], "gt" + lx, "gT" + sfx, 0, DVE)
                tr_to(ot[lb], oT[b], "ot" + lx, "oT" + sfx, 1, DVE)
                lin(gT[b], Wso, "gT" + sfx, "Wso", (2, 3))
                for hf in range(2):
                    P.add(DVE, (lambda e, hf=hf: e.tensor_tensor(out=m1[b][:, hf * 512:(hf + 1) * 512], in0=psb[2 + hf][:, :],
                                                                in1=sgl[lb][:, hf * 512:(hf + 1) * 512], op=ALU.mult)),
                          reads=[PK[2 + hf], "sgl" + lx], writes=[PK[2 + hf], f"m1{hf}" + sfx])
                lin(oT[b], Wdo, "oT" + sfx, "Wdo", (4, 5))
                for hf in range(2):
                    P.add(DVE, (lambda e, hf=hf: e.tensor_tensor(out=mg_[b][:, hf * 512:(hf + 1) * 512], in0=psb[4 + hf][:, :],
                                                                in1=sgl[lb][:, D + hf * 512:D + (hf + 1) * 512], op=ALU.mult)),
                          reads=[PK[4 + hf], "sgl" + lx], writes=[PK[4 + hf], f"mgA{hf}" + sfx])
                    P.add(DVE, (lambda e, hf=hf: e.tensor_tensor(out=mg_[b][:, hf * 512:(hf + 1) * 512], in0=mg_[b][:, hf * 512:(hf + 1) * 512],
                                                                 in1=m1[b][:, hf * 512:(hf + 1) * 512], op=ALU.add)),
                          reads=[f"mgA{hf}" + sfx, f"m1{hf}" + sfx], writes=[f"mg{hf}" + sfx])

            def back_a(i):
                b = i % NB3
                rb = i % NL3
                sfx = f"_{b}"
                rx = f"_R{rb}"
                r0 = 128 * i
                if debug == "merged":
                    P.add(SP, (lambda e: e.dma_start(out=dbgb_d[16 + r0:16 + r0 + 128, :], in_=mg_[b])), reads=["mg0" + sfx, "mg1" + sfx], writes=["dbg"], dma=True)
                tr_to(mg_[b], mT[b], ["mg0" + sfx, "mg1" + sfx], "mT" + sfx, 0, DVE)
                lin(mT[b], Wo, "mT" + sfx, "Wo", (6, 7))
                for hf in range(2):
                    P.add(DVE, (lambda e, hf=hf: e.scalar_tensor_tensor(out=rr[rb][:, hf * 512:(hf + 1) * 512], in0=rr[rb][:, hf * 512:(hf + 1) * 512], scalar=ALPHA,
                                                                       in1=psb[6 + hf][:, :], op0=ALU.mult, op1=ALU.add)),
                          reads=[PK[6 + hf], "rr" + rx], writes=[PK[6 + hf], f"rr{hf}" + rx])
                layer_norm_tile(rr[rb], 128, l1g, l1b, rr[rb], ["rr0" + rx, "rr1" + rx], "h1" + rx, f"ln1{rb}", stat3[rb], ["l1g", "l1b"], eng2=DVE)
                hk = "h1" + rx
                P.add(SP, (lambda e: e.dma_start(out=h1_scr[r0:r0 + 128, :], in_=rr[rb])), reads=[hk], writes=[f"h1s{i}"], dma=True)
                if debug == "h1":
                    P.add(SP, (lambda e: e.dma_start(out=dbg_d[16 + r0:16 + r0 + 128, :], in_=rr[rb])), reads=[hk], writes=["dbg"], dma=True)
                P.add(POOL, (lambda e: e.tensor_copy(out=h1bf[rb], in_=rr[rb])), reads=[hk], writes=["h1bf" + rx])

            def back_b(i):
                rb = i % NL3
                rx = f"_R{rb}"
                sfx = rx
                b = rb
                r0 = 128 * i
                hk = "h1" + rx
                for hf in range(2):
                    def trh(e, hf=hf):
                        ins = None
                        for c in range(4):
                            ins = e.transpose(out=psb[6 + hf][:, c * 128:(c + 1) * 128], in_=rr[rb][:, (4 * hf + c) * 128:(4 * hf + c + 1) * 128], identity=ident)
                        return ins
                    P.add(PE, trh, reads=[hk, "consts"], writes=[PK[6 + hf]])
                    P.add(ACT, (lambda e, hf=hf: e.copy(out=h1T[hf].rearrange("p k t -> p (k t)"), in_=psb[6 + hf][:, :])), reads=[PK[6 + hf]], writes=[PK[6 + hf], f"h1T{hf}"])

                    def mr(e, hf=hf):
                        ins = None
                        for c in range(4):
                            ins = e.matmul(psb[1][:, 0:NEXP], lhsT=h1T[hf][:, c, :], rhs=wr[:, 4 * hf + c, :], start=(hf == 0 and c == 0), stop=False)
                        if hf == 1:
                            ins = e.matmul(psb[1][:, 0:NEXP], lhsT=ones[0:1, :], rhs=brt[0:1, :], start=False, stop=True)
                        return ins
                    P.add(PE, mr, reads=[f"h1T{hf}", "wr", "brt", "consts"], writes=[PK[1]])
                R = rt[b]
                lg, v8, msk, sbase = R[:, 0:32], R[:, 32:40], R[:, 40:72], R[:, 72:104]
                nv0, ex4, gsum, destf, j32 = R[:, 104:105], R[:, 105:109], R[:, 109:110], R[:, 110:114], R[:, 128:160]
                rk = "rt" + sfx
                P.add(DVE, (lambda e: e.tensor_copy(out=lg, in_=psb[1][:, 0:NEXP])), reads=[PK[1]], writes=[PK[1], rk + "lg"])
                if debug == "logits":
                    P.add(SP, (lambda e: e.dma_start(out=dbg_d[16 + r0:16 + r0 + 128, 0:32], in_=lg)), reads=[rk + "lg"], writes=["dbg"], dma=True)
                P.add(DVE, (lambda e: e.max(out=v8, in_=lg)), reads=[rk + "lg"], writes=[rk + "v8"])
                P.add(DVE, (lambda e: e.tensor_scalar(out=msk, in0=lg, scalar1=v8[:, 3:4], scalar2=None, op0=ALU.is_ge)),
                      reads=[rk + "lg", rk + "v8"], writes=[rk + "msk"])

                def mpos(e):
                    e.matmul(psb[1][:, 64:96], lhsT=m_lt, rhs=msk, start=True, stop=True)
                    return e.matmul(psb[1][:, 96:128], lhsT=ones, rhs=msk, start=True, stop=True)
                P.add(PE, mpos, reads=[rk + "msk", "consts"], writes=[PK[1]])
                P.add(DVE, (lambda e: e.tensor_tensor(out=sbase, in0=psb[1][:, 64:96], in1=cnt_eb, op=ALU.add)),
                      reads=[PK[1], "cnt_eb"], writes=[PK[1], rk + "sb0"])
                P.add(DVE, (lambda e: e.tensor_tensor(out=sbase, in0=sbase, in1=emax, op=ALU.min)), reads=[rk + "sb0", "consts"], writes=[rk + "sb"])
                P.add(DVE, (lambda e: e.tensor_tensor(out=cnt_eb, in0=psb[1][:, 96:128], in1=cnt_eb, op=ALU.add)), reads=[PK[1], "cnt_eb"], writes=[PK[1], "cnt_eb"])
                P.add(DVE, (lambda e: e.tensor_scalar(out=nv0, in0=v8[:, 0:1], scalar1=-1.0, scalar2=None, op0=ALU.mult)), reads=[rk + "v8"], writes=[rk + "nv0"])
                P.add(ACT, (lambda e: e.activation(out=ex4, in_=v8[:, 0:4], func=AF.Exp, bias=nv0, scale=1.0, accum_out=gsum)),
                      reads=[rk + "v8", rk + "nv0"], writes=[rk + "ex4", rk + "gsum"])
                P.add(DVE, (lambda e: e.reciprocal(out=gsum, in_=gsum)), reads=[rk + "gsum"], writes=[rk + "grs"])
                P.add(DVE, (lambda e: e.tensor_scalar(out=gate_all[:, i, :], in0=ex4, scalar1=gsum, scalar2=None, op0=ALU.mult)),
                      reads=[rk + "ex4", rk + "grs"], writes=[f"gate{i}"])
                for k in range(4):
                    P.add(DVE, (lambda e, k=k: e.scalar_tensor_tensor(
                        out=j32, in0=lg, scalar=v8[:, k:k + 1], in1=sbase, op0=ALU.is_equal, op1=ALU.mult, accum_out=destf[:, k:k + 1])),
                        reads=[rk + "lg", rk + "v8", rk + "sb"], writes=[rk + "j32", rk + f"df{k}"])
                P.add(DVE, (lambda e: e.tensor_copy(out=dest_all[:, i, :], in_=destf)), reads=[rk + f"df{k}" for k in range(4)], writes=[f"dest{i}"])
                M4 = meta4[b]
                P.add(DVE, (lambda e: e.tensor_scalar(out=M4[:, :, 0], in0=vcol.to_broadcast([128, 4]), scalar1=float(-128 * i), scalar2=None, op0=ALU.add)),
                      reads=["consts"], writes=["meta4" + sfx])
                P.add(DVE, (lambda e: e.tensor_copy(out=M4[:, :, 1], in_=gate_all[:, i, :])), reads=[f"gate{i}"], writes=["meta4" + sfx])
                for k in range(4):
                    P.add(POOL, (lambda e, k=k: e.indirect_dma_start(out=xs_scr, out_offset=IndirectOffsetOnAxis(ap=dest_all[:, i, k:k + 1], axis=0),
                                                                    in_=h1bf[b][:, :], in_offset=None)),
                          reads=[f"dest{i}", "h1bf" + sfx], writes=[f"xs{i}_{k}"], dma=True)
                    P.add(POOL, (lambda e, k=k: e.indirect_dma_start(out=meta_scr, out_offset=IndirectOffsetOnAxis(ap=dest_all[:, i, k:k + 1], axis=0),
                                                                    in_=M4[:, k, :], in_offset=None)),
                          reads=[f"dest{i}", "meta4" + sfx], writes=[f"xm{i}_{k}"], dma=True)

            loads3(0)
            loads3(1)
            loads3(2)
            front3(0)
            front3(1)
            for i in range(32):
                if i + 3 < 32:
                    loads3(i + 3)
                if i + 2 < 32:
                    front3(i + 2)
                if i >= 1:
                    back_b(i - 1)
                back_a(i)
            back_b(31)
            if debug == "route":
                P.add(SP, lambda e: e.dma_start(out=dbg_d[0:128, 0:128], in_=gate_all.rearrange("p a b -> p (a b)")), reads=[f"gate{i}" for i in range(32)], writes=["dbg"], dma=True)
                dtmp = AR.alloc([128, 128])
                P.add(DVE, lambda e: e.tensor_copy(out=dtmp, in_=dest_all.rearrange("p a b -> p (a b)")), reads=[f"dest{i}" for i in range(32)], writes=["dtmp"])
                P.add(SP, lambda e: e.dma_start(out=dbg_d[128:256, 0:128], in_=dtmp), reads=["dtmp"], writes=["dbg"], dma=True)
            barrier()

        if stop_after >= 4:
            AR.reset(0)
            NSTG = 3
            stage = [AR.alloc([128, 8, 256]) for _ in range(NSTG)]
            Wgu = [AR.alloc([128, 8, 2 * D], BF16) for _ in range(2)]
            Wd = [AR.alloc([128, 8, D], BF16) for _ in range(2)]
            bguT = AR.alloc([128, NEXP * 16])
            bguT1 = AR.alloc([128, NEXP * 16])
            P.add(SP, lambda e: e.dma_start(out=bguT, in_=bgu_d), writes=["bguT"], dma=True)
            P.add(DVE, lambda e: e.tensor_scalar(out=bguT1, in0=bguT, scalar1=1.0, scalar2=None, op0=ALU.add), reads=["bguT"], writes=["bguT1"])
            bdb = AR.alloc([128, D], BF16)
            P.add(DVE, lambda e: e.memset(bdb, 0.0), writes=["bdb"])
            P.add(POOL, lambda e: e.dma_start(out=bdb[0:NEXP, :], in_=bd_d), writes=["bdb"], dma=True)
            P.add(DVE, lambda e: e.tensor_scalar(out=bdb[0:NEXP, :], in0=bdb[0:NEXP, :], scalar1=1.702, scalar2=None, op0=ALU.mult), reads=["bdb"], writes=["bdb"])
            selb = AR.alloc([128, NEXP * 128], BF16)
            P.add(DVE, lambda e: e.memset(selb, 0.0), writes=["selb"])
            for q4 in range(4):
                P.add(POOL, (lambda e, q4=q4: e.dma_start(out=selb[0:NEXP, q4 * 1024:(q4 + 1) * 1024], in_=sel_d[:, q4 * 1024:(q4 + 1) * 1024])), writes=["selb"], dma=True)
            xsT = [AR.alloc([128, 8, CAP], BF16) for _ in range(2)]
            xtm = [AR.alloc([128, D], BF16) for _ in range(2)]
            actT = AR.alloc([128, 8, CAP], BF16)
            NBS = 2
            gbt = [AR.alloc([128, 512]) for _ in range(NBS)]
            sgm = [AR.alloc([128, 512]) for _ in range(NBS)]
            u1t = [AR.alloc([128, 512]) for _ in range(NBS)]
            yout = [AR.alloc([128, D]) for _ in range(2)]
            mtl = [AR.alloc([128, 8]) for _ in range(3)]
            mti = [AR.alloc([128, 2], I32) for _ in range(3)]
            NBLK = CAP // 128
            xctr = [0]

            def weight_tasks(e_):
                wb = e_ % 2
                tasks = []
                chunks = []
                for q in range(8):
                    chunks.append((wgu_d, Wgu, q, 1, 2 * D, "Wgu"))
                for q in range(4):
                    chunks.append((wd_d, Wd, 2 * q, 2, D, "Wd"))
                binfo = {}

                def dma_part(k):
                    src, dstl, k0, nk, width, key = chunks[k]
                    bq = stg_ctr[0] % NSTG
                    stg_ctr[0] += 1
                    sv = stage[bq].rearrange("p k c -> p (k c)").rearrange("p (k c) -> p k c", k=nk)
                    binfo[k] = (bq, sv)
                    P.add(SP, (lambda e: e.dma_start(out=sv, in_=src[e_, k0 * 128:(k0 + nk) * 128, :].rearrange("(k p) c -> p k c", p=128))),
                          writes=[f"stg{bq}"], dma=True)

                def cast_part(k):
                    src, dstl, k0, nk, width, key = chunks[k]
                    bq, sv = binfo[k]
                    P.add(ACT, (lambda e: e.copy(out=dstl[wb][:, k0:k0 + nk, :], in_=sv)), reads=[f"stg{bq}"], writes=[f"{key}{wb}"])
                nch = len(chunks)
                for k in range(nch):
                    def task(k=k):
                        dma_part(k)
                        cast_part(k)
                    tasks.append(task)
                return tasks

            def prep_tasks(e_):
                xb = e_ % 2
                tasks = []
                for blk in range(NBLK):
                    def task(blk=blk, e_=e_, xb=xb):
                        tb = xctr[0] % 2
                        xctr[0] += 1
                        s0 = e_ * CAP + blk * 128
                        P.add(ACT, (lambda e: e.dma_start(out=xtm[tb], in_=xs_scr[s0:s0 + 128, :])), writes=[f"xtm{tb}"], dma=True)

                        def trx(e):
                            ins = None
                            pv = psbf(0)
                            for c in range(8):
                                ins = e.transpose(out=pv[:, c * 128:(c + 1) * 128], in_=xtm[tb][:, c * 128:(c + 1) * 128], identity=ident_bf[:, :])
                            return ins
                        P.add(PE, trx, reads=[f"xtm{tb}", "ident_bf"], writes=[PK[0]])
                        P.add(DVE, (lambda e: e.tensor_copy(out=xsT[xb][:, :, blk * 128:(blk + 1) * 128], in_=psbf(0).rearrange("p (k t) -> p k t", k=8))),
                              reads=[PK[0]], writes=[PK[0], f"xsT{xb}"])
                    tasks.append(task)
                return tasks

            for t_ in weight_tasks(0) + prep_tasks(0):
                t_()
            dct = 0
            itc = 0
            for e_ in range(NEXP):
                wb = e_ % 2
                xb = e_ % 2
                bg_tasks = []
                if e_ + 1 < NEXP:
                    wt_, pt_ = weight_tasks(e_ + 1), prep_tasks(e_ + 1)
                    bg_tasks = wt_[0:4] + pt_[0:3] + wt_[4:8] + pt_[3:6] + wt_[8:12]
                for sgi, (s0, N) in enumerate(((0, 512), (512, CAP - 512))):
                    ak = f"actT{sgi}"
                    for c in range(8):
                        pa = 1 + 2 * (itc % 2)
                        tb = itc % NBS
                        itc += 1

                        def mgu(e, c=c, pa=pa, wb=wb, xb=xb, s0=s0, N=N):
                            ins = None
                            for half in range(2):
                                for kc in range(8):
                                    ins = e.matmul(psb[pa + half][:, 0:N], lhsT=Wgu[wb][:, kc, half * D + c * 128:half * D + (c + 1) * 128],
                                                   rhs=xsT[xb][:, kc, s0:s0 + N], start=(kc == 0), stop=(kc == 7))
                            return ins
                        P.add(PE, mgu, reads=[f"Wgu{wb}", f"xsT{xb}"], writes=[PK[pa], PK[pa + 1]])
                        P.add(DVE, (lambda e, pa=pa, tb=tb, N=N, e_=e_, c=c: e.tensor_scalar(out=gbt[tb][:, 0:N], in0=psb[pa][:, 0:N], scalar1=bguT[:, e_ * 16 + c:e_ * 16 + c + 1],
                                                                                             scalar2=7.0, op0=ALU.add, op1=ALU.min)),
                              reads=[PK[pa], "bguT"], writes=[PK[pa], f"gbt{tb}"])
                        P.add(ACT, (lambda e, tb=tb, N=N: e.activation(out=sgm[tb][:, 0:N], in_=gbt[tb][:, 0:N], func=AF.Silu, scale=1.702)),
                              reads=[f"gbt{tb}"], writes=[f"sgm{tb}"])
                        P.add(DVE, (lambda e, pa=pa, tb=tb, N=N, e_=e_, c=c: e.tensor_scalar(out=u1t[tb][:, 0:N], in0=psb[pa + 1][:, 0:N], scalar1=bguT1[:, e_ * 16 + 8 + c:e_ * 16 + 8 + c + 1],
                                                                                             scalar2=8.0, op0=ALU.add, op1=ALU.min)),
                              reads=[PK[pa + 1], "bguT1"], writes=[PK[pa + 1], f"u1t{tb}"])
                        P.add(DVE, (lambda e, tb=tb, N=N, c=c, s0=s0: e.scalar_tensor_tensor(out=actT[:, c, s0:s0 + N], in0=u1t[tb][:, 0:N], scalar=-6.0, in1=sgm[tb][:, 0:N],
                                                                                            op0=ALU.max, op1=ALU.mult)),
                              reads=[f"u1t{tb}", f"sgm{tb}"], writes=[ak])
                        if bg_tasks:
                            bg_tasks.pop(0)()
                    for blk in range(s0 // 128, (s0 + N) // 128):
                        yb = blk % 2
                        mb = dct % 3
                        sl0 = e_ * CAP + blk * 128
                        P.add(ACT, (lambda e, mb=mb, sl0=sl0: e.dma_start(out=mtl[mb][:, 0:2], in_=meta_scr[sl0:sl0 + 128, :])), writes=[f"mtl{mb}"], dma=True)
                        P.add(DVE, (lambda e, mb=mb: e.tensor_scalar(out=mti[mb][:, 0:1], in0=mtl[mb][:, 0:1], scalar1=-1.0, scalar2=float(SEQ), op0=ALU.mult, op1=ALU.add)),
                              reads=[f"mtl{mb}"], writes=[f"mti{mb}"])
                        P.add(DVE, (lambda e, mb=mb: e.tensor_scalar(out=mtl[mb][:, 2:3], in0=mtl[mb][:, 1:2], scalar1=1.0 / 1.702, scalar2=None, op0=ALU.mult)),
                              reads=[f"mtl{mb}"], writes=[f"mtg{mb}"])
                        for half in range(2):
                            bank = 5 + dct % 3
                            dct += 1

                            def mdn(e, blk=blk, half=half, bank=bank, wb=wb, e_=e_):
                                for c in range(8):
                                    e.matmul(psb[bank][:, :], lhsT=actT[:, c, blk * 128:(blk + 1) * 128], rhs=Wd[wb][:, c, half * 512:(half + 1) * 512],
                                             start=(c == 0), stop=False)
                                return e.matmul(psb[bank][:, :], lhsT=selb[:, e_ * 128:(e_ + 1) * 128], rhs=bdb[:, half * 512:(half + 1) * 512], start=False, stop=True)
                            P.add(PE, mdn, reads=[ak, f"Wd{wb}", "selb", "bdb"], writes=[PK[bank]])
                            P.add(ACT, (lambda e, yb=yb, half=half, bank=bank, mb=mb: e.activation(out=yout[yb][:, half * 512:(half + 1) * 512], in_=psb[bank][:, :], func=AF.Copy,
                                                                                                     scale=mtl[mb][:, 2:3])),
                                  reads=[PK[bank], f"mtg{mb}"], writes=[PK[bank], f"yout{yb}"])
                        P.add(POOL, (lambda e, yb=yb, mb=mb: e.indirect_dma_start(out=acc_scr, out_offset=IndirectOffsetOnAxis(ap=mti[mb][:, 0:1], axis=0),
                                                                                 in_=yout[yb][:, :], in_offset=None, compute_op=ALU.add)),
                              reads=[f"yout{yb}", f"mti{mb}"], writes=["acc"], dma=True)
                        if bg_tasks:
                            bg_tasks.pop(0)()
                while bg_tasks:
                    bg_tasks.pop(0)()
            barrier()

        if stop_after >= 5:
            AR.reset(0)
            l2g = AR.alloc([128, D])
            l2b = AR.alloc([128, D])
            bcast_load(l2g, ln2g_d, "l2g")
            bcast_load(l2b, ln2b_d, "l2b")
            NB5 = 4
            hh = [AR.alloc([128, D]) for _ in range(NB5)]
            tt = [AR.alloc([128, D]) for _ in range(NB5)]
            stat5 = [AR.alloc([128, 16]) for _ in range(NB5)]
            for i in range(32):
                b = i % NB5
                sfx = f"_{b}"
                r0 = 128 * i
                P.add(SP, (lambda e, b=b, r0=r0: e.dma_start(out=hh[b], in_=h1_scr[r0:r0 + 128, :])), reads=[f"h1s{i}"], writes=["hh" + sfx], dma=True)
                P.add(SP, (lambda e, b=b, r0=r0: e.dma_start(out=tt[b], in_=acc_scr[r0:r0 + 128, :])), reads=["acc"], writes=["tt" + sfx], dma=True)
                P.add(DVE, (lambda e, b=b: e.scalar_tensor_tensor(out=hh[b], in0=hh[b], scalar=ALPHA, in1=tt[b], op0=ALU.mult, op1=ALU.add)),
                      reads=["hh" + sfx, "tt" + sfx], writes=["hh" + sfx])
                layer_norm_tile(hh[b], 128, l2g, l2b, hh[b], ["hh" + sfx], "ho" + sfx, f"ln2{b}", stat5[b], ["l2g", "l2b"], eng2=DVE, eng3=POOL)
                P.add(POOL, (lambda e, b=b, r0=r0: e.dma_start(out=out_d[r0:r0 + 128, :], in_=hh[b])), reads=["ho" + sfx], writes=[f"out{i}"], dma=True)

        sems = {}
        for e_ in (PE, ACT, DVE, POOL, SP):
            sems[e_] = es.enter_context(nc.semaphore(f"s_{e_}"))
        for e_ in (SP, POOL, ACT):
            for i in range(P.n_dma[e_]):
                sems[(e_, i)] = es.enter_context(nc.semaphore(f"d_{e_}{i}"))
        es.enter_context(nc.allow_low_precision(reason="bf16 matmul operands, fp32 accumulation"))
        block = es.enter_context(nc.Block())
        P.emit(block, sems)

        @block.tensor
        def _(eng):
            P._run(PE, eng)

        @block.scalar
        def _(eng):
            P._run(ACT, eng)

        @block.vector
        def _(eng):
            P._run(DVE, eng)

        @block.gpsimd
        def _(eng):
            P._run(POOL, eng)

        @block.sync
        def _(eng):
            P._run(SP, eng)
            final = {}
            for op in P.ops:
                if op.dma:
                    final[id(op.sem)] = (op.sem, max(final.get(id(op.sem), (None, 0))[1], op.val))
            for s, v in final.values():
                eng.wait_ge(s, v)
    return nc


def make_consts():
    k = np.arange(128)[:, None]
    l = np.arange(128)[None, :]
    c = np.zeros((128, 5 * 128 + 72), np.float32)
    c[:, 704] = SEQ - np.arange(128)
    c[:, 640:672] = np.arange(NEXP)[None, :] * CAP
    c[:, 672:704] = np.arange(NEXP)[None, :] * CAP + CAP - 1
    c[:, 0:128] = (k == l)
    c[:, 128:256] = (k <= l)
    c[:, 256:384] = (k > l)
    c[:, 384:512] = (k < l)
    c[:, 512:640] = 1.0
    return c


def make_sel():
    s_ = np.zeros((NEXP, NEXP, 128), np.float32)
    for e_ in range(NEXP):
        s_[e_, e_, :] = 1.0
    return s_.reshape(NEXP, NEXP * 128)


def make_attc():
    a = np.zeros((1, 1024), np.float32)
    a[0, 64:128] = -30000.0
    a[0, 128:640] = 1.0
    return a


def make_inputs(inputs, b):
    f = lambda a: np.ascontiguousarray(np.asarray(a, dtype=np.float32))
    cw = f(inputs["conv_w"])[0]
    cb = f(inputs["conv_b"])[0]
    return {
        "x": f(inputs["x"][b]),
        "meta": f(inputs["meta_tokens"]),
        "consts": make_consts(),
        "ln_in_g": f(inputs["ln_in_g"])[None],
        "ln_in_b": f(inputs["ln_in_b"])[None],
        "w_in": f(inputs["w_in"])[0],
        "convw_t": f(cw.reshape(4, 16, 128).transpose(2, 0, 1).reshape(128, 64)),
        "convb_t": f(cb.reshape(16, 128).T),
        "dt_bias": f(inputs["dt_bias"]),
        "a_log": f(inputs["a_log"]),
        "d_skip": f(inputs["d_skip"]),
        "ssd_norm_w": f(inputs["ssd_norm_w"]),
        "lam": f(np.concatenate([inputs["lam_q1"], inputs["lam_k1"], inputs["lam_q2"], inputs["lam_k2"]], axis=0)),
        "subln_w": f(inputs["subln_w"]),
        "attc": make_attc(),
        "b_gate": f(inputs["b_gate"]),
        "w_ssd_out": f(inputs["w_ssd_out"])[0],
        "w_da_out": f(inputs["w_da_out"])[0],
        "w_out": f(inputs["w_out"])[0],
        "ln1_g": f(inputs["ln1_g"]),
        "ln1_b": f(inputs["ln1_b"]),
        "w_router": f(inputs["w_router"])[0],
        "b_router": f(inputs["b_router"]),
        "w_gate_up": f(inputs["w_gate_up"])[0],
        "bgu_t": f(f(inputs["b_gate_up"])[0].reshape(NEXP, 16, 128).transpose(2, 0, 1).reshape(128, NEXP * 16)),
        "w_down": f(inputs["w_down"])[0],
        "b_down": f(inputs["b_down"])[0],
        "sel": make_sel(),
        "ln2_g": f(inputs["ln2_g"]),
        "ln2_b": f(inputs["ln2_b"]),
    }


_NC_CACHE = {}


def kernel(**inputs):
    n = 8
    if "nc" not in _NC_CACHE:
        _NC_CACHE["nc"] = build_program()
    nc = _NC_CACHE["nc"]
    shared = make_inputs(inputs, 0)
    in_maps = []
    for b in range(n):
        m = dict(shared)
        m["x"] = np.ascontiguousarray(np.asarray(inputs["x"][b], dtype=np.float32))
        in_maps.append(m)
    res = run_bass_kernel_spmd(nc, in_maps, core_ids=list(range(n)))
    out = np.stack([np.asarray(r["out"], dtype=np.float32) for r in res.results], axis=0)
    return out
```

```python
import numpy as np
import concourse.bass as bass
import concourse.mybir as mybir
from concourse.bass import IndirectOffsetOnAxis
from concourse.bass_utils import run_bass_kernel_spmd

F32 = mybir.dt.float32
BF16 = mybir.dt.bfloat16
I32 = mybir.dt.int32
U32 = mybir.dt.uint32
ALU = mybir.AluOpType
AF = mybir.ActivationFunctionType
AX = mybir.AxisListType

PE, ACT, DVE, POOL, SP = "pe", "act", "dve", "pool", "sp"
ENGS = (PE, ACT, DVE, POOL, SP)

D = 1024
SEQ = 4096
NMETA = 16
T = SEQ + NMETA
NTILE = 33
INCOLS = 8208
NEXP = 32
CAP = 768
NSLOT = NEXP * CAP
ALPHA = 2.0 ** 0.25
LN_EPS = 1e-5
RMS_EPS = 1e-6
LAMBDA_INIT = 0.2
C_Z, C_XBC, C_DT, C_Q, C_K, C_V, C_G = 0, 1024, 3072, 3088, 4112, 5136, 6160


def tcol(i):
    return (0, 16) if i == 0 else (16 + 128 * (i - 1), 128)


class Op:
    __slots__ = ("eng", "fn", "dma", "deps", "sig", "sem", "val", "prev", "name")


class Prog:
    def __init__(self, nc, n_sp=32, n_pool=16, n_act=40):
        self.nc = nc
        self.ops = []
        self.last_w = {}
        self.readers = {}
        self.n_dma = {SP: n_sp, POOL: n_pool, ACT: n_act}
        self.canon = {}

    def alias(self, *keys):
        base = self.canon.get(keys[0], keys[0])
        for k in keys[1:]:
            old = self.canon.get(k, k)
            if old == base:
                continue
            assert old not in self.last_w and old not in self.readers, old
            for kk, vv in list(self.canon.items()):
                if vv == old:
                    self.canon[kk] = base
            self.canon[k] = base

    def add(self, eng, fn, reads=(), writes=(), dma=False, name=""):
        op = Op()
        op.eng, op.fn, op.dma, op.name = eng, fn, dma, name
        op.sig = dma
        reads = list(dict.fromkeys(self.canon.get(k, k) for k in reads))
        writes = list(dict.fromkeys(self.canon.get(k, k) for k in writes))
        deps = []
        for k in reads:
            w = self.last_w.get(k)
            if w is not None:
                deps.append(w)
        for k in writes:
            w = self.last_w.get(k)
            if w is not None and (w.dma or dma or w.eng != eng):
                deps.append(w)
            for r in self.readers.get(k, ()):
                if r is not op and (r.dma or dma or r.eng != eng):
                    deps.append(r)
        seen = set()
        op.deps = []
        for d_ in deps:
            if id(d_) not in seen:
                seen.add(id(d_))
                op.deps.append(d_)
                d_.sig = True
        for k in writes:
            self.last_w[k] = op
            self.readers[k] = []
        for k in reads:
            lst = self.readers.setdefault(k, [])
            if not dma:
                lst[:] = [r for r in lst if r.dma or r.eng != eng]
            lst.append(op)
        self.ops.append(op)
        return op

    def barrier(self, fence_fn):
        allkeys = [k for k in (set(self.last_w) | set(self.readers)) if not k.startswith("z_")]
        self.add(DVE, fence_fn, writes=allkeys + ["fence"])
        for e_ in (PE, ACT, POOL, SP):
            self.add(e_, (lambda e: e.nop()), reads=["fence"])

    def emit(self, block, sems):
        cnt = {e: 0 for e in ENGS}
        dcount = {}
        dlast = {}
        for op in self.ops:
            if op.dma:
                n = self.n_dma[op.eng]
                i = dcount.get(op.eng, 0)
                dcount[op.eng] = i + 1
                s = sems[(op.eng, i % n)]
                op.sem = s
                op.prev = dlast.get(id(s), 0)
                op.val = op.prev + 16
                dlast[id(s)] = op.val
            elif op.sig:
                cnt[op.eng] += 1
                op.sem = sems[op.eng]
                op.val = cnt[op.eng]
        self.maxcnt = dict(cnt)
        per = {e: [o for o in self.ops if o.eng == e] for e in ENGS}

        def run(eng_name, eng):
            known = {}
            for op in per[eng_name]:
                need = {}
                for d_ in op.deps:
                    k = id(d_.sem)
                    if need.get(k, (None, 0))[1] < d_.val:
                        need[k] = (d_.sem, d_.val)
                if op.dma and op.prev > 0:
                    k = id(op.sem)
                    if need.get(k, (None, 0))[1] < op.prev:
                        need[k] = (op.sem, op.prev)
                for k, (s, v) in need.items():
                    if known.get(k, 0) < v:
                        eng.wait_ge(s, v)
                        known[k] = v
                ins = op.fn(eng)
                if op.dma:
                    ins.then_inc(op.sem, 16)
                elif op.sig:
                    ins.then_inc(op.sem, 1)
            if eng_name == SP:
                for k, v in dlast.items():
                    pass

        self._run = run
        self._dlast = dlast
        return per


class Arena:
    def __init__(self, ap, words):
        self.ap, self.words, self.off = ap, words, 0

    def reset(self, keep=0):
        self.off = keep

    def alloc(self, shape, dt=F32):
        n = 1
        for s_ in shape[1:]:
            n *= s_
        words = n if dt in (F32, I32, U32) else (n + 1) // 2
        words = (words + 7) // 8 * 8
        assert self.off + words <= self.words, (self.off, words, self.words)
        sl = self.ap[:, self.off:self.off + words]
        self.off += words
        if dt != F32:
            sl = sl.bitcast(dt)
        sl = sl[:, 0:n]
        if len(shape) == 3:
            sl = sl.rearrange("p (a b) -> p a b", a=shape[1])
        elif len(shape) == 4:
            sl = sl.rearrange("p (a b c) -> p a b c", a=shape[1], b=shape[2])
        return sl


H0T_WORDS = 8 * T // 2
ARENA_WORDS = 35000 + H0T_WORDS


def build_program(debug=None, stop_after=99):
    nc = bass.Bass("TRN2", target_bir_lowering=False)
    P = Prog(nc)

    def din(name, shape, dt=F32):
        return nc.dram_tensor(name, list(shape), dt, kind="ExternalInput").ap()

    x_d = din("x", [SEQ, D])
    meta_d = din("meta", [NMETA, D])
    consts_d = din("consts", [128, 5 * 128 + 72])
    lnin_g_d = din("ln_in_g", [1, D])
    lnin_b_d = din("ln_in_b", [1, D])
    w_in_d = din("w_in", [D, INCOLS])
    convw_d = din("convw_t", [128, 64])
    convb_d = din("convb_t", [128, 16])
    dtb_d = din("dt_bias", [1, 16])
    alog_d = din("a_log", [1, 16])
    dsk_d = din("d_skip", [1, 16])
    nw_d = din("ssd_norm_w", [1, D])
    lam_d = din("lam", [4, 64])
    subln_d = din("subln_w", [1, 128])
    attc_d = din("attc", [1, 1024])
    bgate_d = din("b_gate", [1, 2048])
    wso_d = din("w_ssd_out", [D, D])
    wdo_d = din("w_da_out", [D, D])
    wo_d = din("w_out", [D, D])
    ln1g_d = din("ln1_g", [1, D])
    ln1b_d = din("ln1_b", [1, D])
    wr_d = din("w_router", [D, NEXP])
    br_d = din("b_router", [1, NEXP])
    wgu_d = din("w_gate_up", [NEXP, D, 2 * D])
    bgu_d = din("bgu_t", [128, NEXP * 16])
    wd_d = din("w_down", [NEXP, D, D])
    bd_d = din("b_down", [NEXP, D])
    sel_d = din("sel", [NEXP, NEXP * 128])
    ln2g_d = din("ln2_g", [1, D])
    ln2b_d = din("ln2_b", [1, D])
    out_d = nc.dram_tensor("out", [SEQ, D], F32, kind="ExternalOutput").ap()
    dbg_d = dbgb_d = None
    if debug:
        dbg_d = nc.dram_tensor("dbg", [T, D], F32, kind="ExternalOutput").ap()
        dbgb_d = nc.dram_tensor("dbgb", [T, D], BF16, kind="ExternalOutput").ap()
    h0_scr = nc.dram_tensor("h0_scr", [T, D], F32).ap()
    g_scr = nc.dram_tensor("g_scr", [SEQ, D], BF16).ap()
    o_scr = nc.dram_tensor("o_scr", [SEQ, D], BF16).ap()
    sg_scr = nc.dram_tensor("sg_scr", [SEQ, 2 * D], BF16).ap()
    h1_scr = nc.dram_tensor("h1_scr", [SEQ, D], F32).ap()
    xs_scr = nc.dram_tensor("xs_scr", [NSLOT, D], BF16).ap()
    acc_scr = nc.dram_tensor("acc_scr", [SEQ + 128, D], F32).ap()
    meta_scr = nc.dram_tensor("meta_scr", [NSLOT, 2], F32).ap()

    import contextlib
    es = contextlib.ExitStack()
    with es:
        def sb(name, shape, dt=F32):
            return es.enter_context(nc.sbuf_tensor("sb_" + name, list(shape), dt))

        def ps(name, shape, dt=F32):
            return es.enter_context(nc.psum_tensor("ps_" + name, list(shape), dt))

        consts = sb("consts", [128, 5 * 128 + 72])
        ident = consts[:, 0:128]
        m_le = consts[:, 128:256]
        m_gt = consts[:, 256:384]
        m_lt = consts[:, 384:512]
        ones = consts[:, 512:640]
        ebase = consts[:, 640:672]
        emax = consts[:, 672:704]
        vcol = consts[:, 704:705]
        ident_bf = sb("ident_bf", [128, 128], BF16)
        small = sb("small", [128, 64])
        eps_t = small[:, 0:1]
        rmseps_t = small[:, 1:2]
        fence_t = small[:, 2:4]
        one_t = small[:, 4:5]
        mhalf_t = small[:, 5:6]
        arena_t = sb("arena", [128, ARENA_WORDS])
        AR = Arena(arena_t[:, :], ARENA_WORDS)
        h0T = AR.alloc([128, 8, T], BF16)
        assert AR.off == H0T_WORDS
        gate_all = sb("gate_all", [128, 32, 4])
        dest_all = sb("dest_all", [128, 32, 4], I32)
        psall = ps("psall", [128, 4096])
        psb = [psall[:, i * 512:(i + 1) * 512] for i in range(8)]
        PK = [f"ps{i}" for i in range(8)]

        def psbf(i):
            return psb[i].bitcast(BF16)

        P.add(SP, lambda e: e.dma_start(out=consts[:], in_=consts_d), writes=["consts"], dma=True)
        P.add(DVE, lambda e: e.tensor_copy(out=ident_bf[:], in_=ident), reads=["consts"], writes=["ident_bf"])
        P.add(DVE, lambda e: e.memset(eps_t, LN_EPS), writes=["eps"])
        P.add(DVE, lambda e: e.memset(rmseps_t, RMS_EPS), writes=["eps"])
        P.add(DVE, lambda e: e.memset(one_t, 1.0), writes=["eps"])
        P.add(DVE, lambda e: e.memset(mhalf_t, -0.5), writes=["eps"])

        def barrier():
            P.barrier(lambda e: e.memset(fence_t, 0.0))

        def bcast_load(dst, src_row, key):
            P.add(SP, lambda e: e.dma_start(out=dst, in_=src_row.partition_broadcast(128)), writes=[key], dma=True)

        def layer_norm_tile(xin, n, gb, bb, out_ap, keys_in, key_out, tag, stat, gbkeys, eng2=POOL, eng3=None, rstd_pool=False):
            eng3 = eng3 or eng2
            st6 = stat[:, 0:12]
            mv = stat[:, 12:14]
            rstd = stat[:, 14:15]
            skey = tag + "_stat"
            P.alias(*(list(keys_in) + [key_out + "_n", key_out + "_g", key_out]))
            P.alias(skey + "r0", skey + "r")
            P.add(DVE, lambda e: e.bn_stats(out=st6[:n, 0:6], in_=xin[:n, 0:512]), reads=keys_in, writes=[skey + "a"])
            P.add(DVE, lambda e: e.bn_stats(out=st6[:n, 6:12], in_=xin[:n, 512:1024]), reads=keys_in, writes=[skey + "b"])
            P.add(DVE, lambda e: e.bn_aggr(out=mv[:n], in_=st6[:n]), reads=[skey + "a", skey + "b"], writes=[skey + "mv"])
            if rstd_pool:
                P.add(POOL, lambda e: e.tensor_scalar(out=rstd[:n], in0=mv[:n, 1:2], scalar1=LN_EPS, scalar2=None, op0=ALU.add),
                      reads=[skey + "mv"], writes=[skey + "r0"])
                P.add(POOL, lambda e: e.tensor_tensor(out=rstd[:n], in0=rstd[:n], in1=mhalf_t[:n], op=ALU.pow),
                      reads=[skey + "r0", "eps"], writes=[skey + "r"])
            else:
                P.add(ACT, lambda e: e.activation(out=rstd[:n], in_=mv[:n, 1:2], func=AF.Ln, bias=eps_t[:n], scale=1.0),
                      reads=[skey + "mv", "eps"], writes=[skey + "r0"])
                P.add(ACT, lambda e: e.activation(out=rstd[:n], in_=rstd[:n], func=AF.Exp, scale=-0.5),
                      reads=[skey + "r0"], writes=[skey + "r"])
            P.add(DVE, lambda e: e.tensor_scalar(out=out_ap[:n], in0=xin[:n], scalar1=mv[:n, 0:1], scalar2=rstd[:n],
                                                 op0=ALU.subtract, op1=ALU.mult),
                  reads=keys_in + [skey + "mv", skey + "r"], writes=[key_out + "_n"])
            P.add(eng2, lambda e: e.tensor_tensor(out=out_ap[:n], in0=out_ap[:n], in1=gb[:n], op=ALU.mult),
                  reads=[key_out + "_n"] + gbkeys, writes=[key_out + "_g"])
            P.add(eng3, lambda e: e.tensor_tensor(out=out_ap[:n], in0=out_ap[:n], in1=bb[:n], op=ALU.add),
                  reads=[key_out + "_g"] + gbkeys, writes=[key_out])

        stg_ctr = [0]

        def load_cast(dst, src2d, ncols, key, stage=None, cast_eng=ACT):
            for c0 in range(0, ncols, 1024):
                c1 = min(ncols, c0 + 1024)
                P.add(POOL, (lambda e, c0=c0, c1=c1: e.dma_start(
                    out=dst[:, :, c0:c1], in_=src2d[:, c0:c1].rearrange("(k p) c -> p k c", p=128))), writes=[key], dma=True)
            return

        def load_cast_staged(dst, src2d, ncols, key, stage, cast_eng=ACT):
            sw = stage[0].shape[2]
            for c0 in range(0, ncols, sw):
                c1 = min(ncols, c0 + sw)
                w = c1 - c0
                b = stg_ctr[0] % len(stage)
                stg_ctr[0] += 1
                sk = f"stg{b}"
                P.add(SP, (lambda e, b=b, c0=c0, c1=c1, w=w: e.dma_start(
                    out=stage[b][:, :, 0:w], in_=src2d[:, c0:c1].rearrange("(k p) c -> p k c", p=128))),
                    writes=[sk], dma=True)
                if cast_eng == ACT:
                    fn = (lambda e, b=b, c0=c0, c1=c1, w=w: e.copy(out=dst[:, :, c0:c1], in_=stage[b][:, :, 0:w]))
                else:
                    fn = (lambda e, b=b, c0=c0, c1=c1, w=w: e.tensor_copy(out=dst[:, :, c0:c1], in_=stage[b][:, :, 0:w]))
                P.add(cast_eng, fn, reads=[sk], writes=[key])

        AR.reset(H0T_WORDS)
        Wssd = AR.alloc([128, 8, 3088], BF16)
        load_cast(Wssd, w_in_d[:, 0:3088], 3088, "Wssd")
        WSSD_END = AR.off
        g_bc = AR.alloc([128, D])
        b_bc = AR.alloc([128, D])
        bcast_load(g_bc, lnin_g_d, "gb0")
        bcast_load(b_bc, lnin_b_d, "gb0b")
        NB0 = 4
        xt = [AR.alloc([128, D]) for i in range(NB0)]
        hb = [AR.alloc([128, D], BF16) for i in range(NB0)]
        stat0 = [AR.alloc([128, 16]) for i in range(NB0)]
        for i in range(NTILE):
            c0, n = tcol(i)
            b = i % NB0
            xk, hk = f"xt{b}", f"hb{b}"
            src = meta_d if i == 0 else x_d[(i - 1) * 128:i * 128, :]
            P.add(SP, (lambda e, b=b, n=n, src=src: e.dma_start(out=xt[b][:n], in_=src)), writes=[xk], dma=True)
            layer_norm_tile(xt[b], n, g_bc, b_bc, xt[b], [xk], xk + "h", f"ln0{b}", stat0[b], ["gb0", "gb0b"], eng2=DVE, eng3=POOL)
            P.add(POOL, (lambda e, b=b, n=n, c0=c0: e.dma_start(out=h0_scr[c0:c0 + n, :], in_=xt[b][:n])),
                  reads=[xk + "h"], writes=[f"h0s{i}"], dma=True)
            if debug == "h0":
                P.add(SP, (lambda e, b=b, n=n, c0=c0: e.dma_start(out=dbg_d[c0:c0 + n, :], in_=xt[b][:n])),
                      reads=[xk + "h"], writes=["dbg"], dma=True)
            P.add(ACT, (lambda e, b=b, n=n: e.copy(out=hb[b][:n], in_=xt[b][:n])), reads=[xk + "h"], writes=[hk])
            pb = i % 2
            pst = psbf(pb)

            def tr(e, b=b, n=n, pst=pst):
                ins = None
                for kc in range(8):
                    ins = e.transpose(out=pst[:, kc * 128:kc * 128 + n], in_=hb[b][:n, kc * 128:(kc + 1) * 128],
                                      identity=ident_bf[:n, :n])
                return ins
            P.add(PE, tr, reads=[hk, "ident_bf"], writes=[PK[pb]])
            P.add(ACT, (lambda e, n=n, c0=c0, pst=pst: e.copy(
                out=h0T[:, :, c0:c0 + n], in_=pst.rearrange("p (k t) -> p k t", k=8)[:, :, 0:n])),
                reads=[PK[pb]], writes=[PK[pb], f"h0T{i}"])
        H0T_ALL = [f"h0T{i}" for i in range(NTILE)]
        barrier()

        if stop_after >= 1:
            AR.reset(WSSD_END)
            convw = AR.alloc([128, 64])
            convb = AR.alloc([128, 16])
            P.add(SP, lambda e: e.dma_start(out=convw, in_=convw_d), writes=["convw"], dma=True)
            P.add(SP, lambda e: e.dma_start(out=convb, in_=convb_d), writes=["convb"], dma=True)
            diagw = AR.alloc([128, 64, 128], BF16)
            for j in range(64):
                P.add(DVE, (lambda e, j=j: e.tensor_scalar(out=diagw[:, j, :], in0=ident, scalar1=convw[:, j:j + 1],
                                                           scalar2=None, op0=ALU.mult)),
                      reads=["consts", "convw"], writes=["diagw"])
            par = AR.alloc([128, 64])
            dtb_bc, alog_bc, dsk_bc, A_bc = par[:, 0:16], par[:, 16:32], par[:, 32:48], par[:, 48:64]
            bcast_load(dtb_bc, dtb_d, "par0")
            bcast_load(alog_bc, alog_d, "par1")
            bcast_load(dsk_bc, dsk_d, "par2")
            P.add(ACT, lambda e: e.activation(out=A_bc, in_=alog_bc, func=AF.Exp), reads=["par1"], writes=["parA0"])
            P.add(DVE, lambda e: e.tensor_scalar(out=A_bc, in0=A_bc, scalar1=-1.0, scalar2=None, op0=ALU.mult),
                  reads=["parA0"], writes=["parA"])
            nw_bc = AR.alloc([128, D])
            bcast_load(nw_bc, nw_d, "nw")
            state = AR.alloc([128, 16, 64])
            state_bf = AR.alloc([128, 16, 64], BF16)
            P.add(DVE, lambda e: e.memset(state, 0.0), writes=["state"])
            P.add(DVE, lambda e: e.memset(state_bf, 0.0), writes=["state_bf"])
            xp = [AR.alloc([128, 515], BF16) for _ in range(2)]
            halo = AR.alloc([128, 16, 3], BF16)
            P.add(DVE, lambda e: e.memset(halo, 0.0), writes=[f"halo{c}" for c in range(16)])
            xact = AR.alloc([128, 16, 512], BF16)
            x_tm = AR.alloc([128, 16, 64], BF16)
            B_tm = AR.alloc([128, 512], BF16)
            szs = [AR.alloc([128, D], BF16) for _ in range(2)]
            sm = AR.alloc([128, 192])
            dtraw, e1, dtt, a_t = sm[:, 0:16], sm[:, 16:32], sm[:, 32:48], sm[:, 48:64]
            cstot, expcs, dte = sm[:, 64:96], sm[:, 96:112], sm[:, 112:128]
            cdec, dtdte, ss, rstd4 = sm[:, 128:144], sm[:, 144:160], sm[:, 160:164], sm[:, 164:168]
            rhsA = AR.alloc([128, 8, 128])
            Eexp = AR.alloc([128, 16, 128], BF16)
            cbm = AR.alloc([128, 4, 128])
            WT = AR.alloc([128, 16, 128], BF16)
            xdt = AR.alloc([128, 16, 64], BF16)
            xdd = AR.alloc([128, 16, 64], BF16)
            xD = AR.alloc([128, 16, 64], BF16)
            t1s = [AR.alloc([128, D]) for _ in range(2)]
            tailq = []
            gout = AR.alloc([128, D], BF16)
            junk = AR.alloc([128, 256])

            for cp_ in range(2):
                P.alias(f"t1_{cp_}", f"t10_{cp_}", f"t11_{cp_}", f"t1b0_{cp_}", f"t1b1_{cp_}", f"gy_{cp_}")
            P.alias("rstd4a", "rstd4")
            P.alias("dte0", "dte")
            groups = [(0, 16, [0])] + [(16 + 512 * g, 512, [1 + 4 * g + j for j in range(4)]) for g in range(8)]
            for gi, (gc0, N, tiles) in enumerate(groups):
                hkeys = [f"h0T{t_}" for t_ in tiles]
                def emit_mm(c):
                    b = c % 2
                    xk = f"xp{b}"

                    def mm(e, c=c, b=b, gc0=gc0, N=N):
                        ins = None
                        for kc in range(8):
                            ins = e.matmul(psb[b][:, 0:N], lhsT=Wssd[:, kc, 1024 + c * 128:1024 + (c + 1) * 128],
                                           rhs=h0T[:, kc, gc0:gc0 + N], start=(kc == 0), stop=(kc == 7))
                        return ins
                    P.add(PE, mm, reads=["Wssd"] + hkeys, writes=[PK[b]])
                    P.add(ACT, (lambda e, b=b, N=N: e.copy(out=xp[b][:, 3:3 + N], in_=psb[b][:, 0:N])),
                          reads=[PK[b]], writes=[PK[b], xk])
                    P.add(POOL, (lambda e, b=b, c=c: e.tensor_copy(out=xp[b][:, 0:3], in_=halo[:, c, :])),
                          reads=[f"halo{c}"], writes=[xk + "h"])

                def emit_cv(c):
                    b = c % 2
                    xk = f"xp{b}"

                    def cv(e, c=c, b=b, N=N):
                        ins = None
                        for kk in range(4):
                            ins = e.matmul(psb[2 + b][:, 0:N], lhsT=diagw[:, kk * 16 + c, :], rhs=xp[b][:, kk:kk + N],
                                           start=(kk == 0), stop=(kk == 3))
                        return ins
                    P.add(PE, cv, reads=["diagw", xk, xk + "h"], writes=[PK[2 + b]])
                    P.add(POOL, (lambda e, b=b, c=c, N=N: e.tensor_copy(out=halo[:, c, :], in_=xp[b][:, N:N + 3])),
                          reads=[xk], writes=[f"halo{c}"])
                    P.add(ACT, (lambda e, c=c, b=b, N=N: e.activation(out=xact[:, c, 0:N], in_=psb[2 + b][:, 0:N], func=AF.Silu,
                                                                     bias=convb[:, c:c + 1], scale=1.0)),
                          reads=[PK[2 + b], "convb"], writes=[PK[2 + b], "xact"])
                emit_mm(0)
                for c in range(16):
                    if c + 1 < 16:
                        emit_mm(c + 1)
                    emit_cv(c)
                for j, ti in enumerate(tiles):
                    off = 0 if ti == 0 else 128 * j
                    tc0, n = tcol(ti)
                    real = ti > 0
                    cp = ti % 2
                    sz = szs[cp]
                    t1 = t1s[cp]
                    gy = t1
                    szk = f"sz{cp}"
                    def trx(e, off=off, n=n):
                        ins = None
                        pv = psbf(0)
                        for c in range(8):
                            ins = e.transpose(out=pv[:n, c * 128:(c + 1) * 128], in_=xact[:, c, off:off + n], identity=ident_bf[:, :])
                        return ins
                    P.add(PE, trx, reads=["xact", "ident_bf"], writes=[PK[0]])
                    P.add(ACT, (lambda e, n=n: e.copy(out=x_tm[:n].rearrange("p h d -> p (h d)"), in_=psbf(0)[:n, :])),
                          reads=[PK[0]], writes=[PK[0], "x_tm"])

                    def trb(e, off=off, n=n):
                        ins = None
                        pv = psbf(1)
                        for c in range(4):
                            ins = e.transpose(out=pv[:n, c * 128:(c + 1) * 128], in_=xact[:, 8 + c, off:off + n], identity=ident_bf[:, :])
                        return ins
                    P.add(PE, trb, reads=["xact", "ident_bf"], writes=[PK[1]])
                    P.add(DVE, (lambda e, n=n: e.tensor_copy(out=B_tm[:n], in_=psbf(1)[:n, 0:512])),
                          reads=[PK[1]], writes=[PK[1], "B_tm"])
                    def mdt(e, tc0=tc0, n=n):
                        ins = None
                        for kc in range(8):
                            ins = e.matmul(psb[1][:n, 256:272], lhsT=h0T[:, kc, tc0:tc0 + n], rhs=Wssd[:, kc, 3072:3088],
                                           start=(kc == 0), stop=(kc == 7))
                        return ins
                    P.add(PE, mdt, reads=["Wssd", f"h0T{ti}"], writes=[PK[1]])
                    P.add(DVE, (lambda e, n=n: e.tensor_tensor(out=dtraw[:n], in0=psb[1][:n, 256:272], in1=dtb_bc[:n], op=ALU.add)),
                          reads=[PK[1], "par0"], writes=[PK[1], "dtraw"])
                    P.add(ACT, (lambda e, n=n: e.activation(out=e1[:n], in_=dtraw[:n], func=AF.Exp)), reads=["dtraw"], writes=["e1"])
                    P.add(ACT, (lambda e, n=n: e.activation(out=dtt[:n], in_=e1[:n], func=AF.Ln, bias=one_t[:n], scale=1.0)),
                          reads=["e1", "eps"], writes=["dtt"])
                    P.add(DVE, (lambda e, n=n: e.tensor_tensor(out=a_t[:n], in0=dtt[:n], in1=A_bc[:n], op=ALU.mult)),
                          reads=["dtt", "parA"], writes=["a_t"])
                    def mcs(e, n=n):
                        e.matmul(psb[1][:n, 272:288], lhsT=m_le[:n, :n], rhs=a_t[:n, :], start=True, stop=True)
                        return e.matmul(psb[1][:, 288:304], lhsT=ones[:n, :], rhs=a_t[:n, :], start=True, stop=True)
                    P.add(PE, mcs, reads=["a_t", "consts"], writes=[PK[1]])
                    P.add(ACT, (lambda e: e.copy(out=cstot[:, :], in_=psb[1][:, 272:304])), reads=[PK[1]], writes=[PK[1], "cstot"])
                    P.add(ACT, (lambda e: e.activation(out=cdec[:, :], in_=cstot[:, 16:32], func=AF.Exp)), reads=["cstot"], writes=["cdec"])
                    P.add(DVE, (lambda e, n=n: e.tensor_tensor(out=dte[:n], in0=cstot[:n, 16:32], in1=cstot[:n, 0:16], op=ALU.subtract)),
                          reads=["cstot"], writes=["dte0"])
                    P.add(ACT, (lambda e, n=n: e.activation(out=dte[:n], in_=dte[:n], func=AF.Exp)), reads=["dte0"], writes=["dte"])
                    P.add(DVE, (lambda e, n=n: e.tensor_tensor(out=dtdte[:n], in0=dte[:n], in1=dtt[:n], op=ALU.mult)),
                          reads=["dte", "dtt"], writes=["dtdte"])
                    P.add(POOL, (lambda e, n=n: e.tensor_tensor(out=xdd[:n], in0=x_tm[:n], in1=dtdte[:n].unsqueeze(2).to_broadcast([n, 16, 64]), op=ALU.mult)),
                          reads=["x_tm", "dtdte"], writes=["xdd"])
                    if real:
                        P.add(ACT, (lambda e, n=n: e.activation(out=expcs[:n], in_=cstot[:n, 0:16], func=AF.Exp)), reads=["cstot"], writes=["expcs"])
                        P.add(DVE, (lambda e, n=n: e.tensor_tensor(out=xdt[:n], in0=x_tm[:n], in1=dtt[:n].unsqueeze(2).to_broadcast([n, 16, 64]), op=ALU.mult)),
                              reads=["x_tm", "dtt"], writes=["xdt"])
                        P.add(POOL, (lambda e, n=n: e.tensor_tensor(out=xD[:n], in0=x_tm[:n], in1=dsk_bc[:n].unsqueeze(2).to_broadcast([n, 16, 64]), op=ALU.mult)),
                              reads=["x_tm", "par2"], writes=["xD"])
                        for hf in range(2):
                            def mz(e, hf=hf, tc0=tc0, n=n):
                                ins = None
                                for kc in range(8):
                                    ins = e.matmul(psb[2 + hf][:n, :], lhsT=h0T[:, kc, tc0:tc0 + n], rhs=Wssd[:, kc, hf * 512:(hf + 1) * 512],
                                                   start=(kc == 0), stop=(kc == 7))
                                return ins
                            P.add(PE, mz, reads=["Wssd", f"h0T{ti}"], writes=[PK[2 + hf]])
                            P.add(ACT, (lambda e, hf=hf, n=n, sz=sz: e.activation(out=sz[:n, hf * 512:(hf + 1) * 512], in_=psb[2 + hf][:n, :], func=AF.Silu)),
                                  reads=[PK[2 + hf]], writes=[PK[2 + hf], szk])
                        for hh2 in range(2):
                            P.add(DVE, (lambda e, n=n, hh2=hh2: e.tensor_tensor(
                                out=rhsA[:n], in0=m_le[:n, :].unsqueeze(1).to_broadcast([n, 8, 128]),
                                in1=a_t[:n, 8 * hh2:8 * hh2 + 8].unsqueeze(2).to_broadcast([n, 8, 128]), op=ALU.mult)),
                                reads=["a_t", "consts"], writes=["rhsA"])
                            for q2 in range(2):
                                q = 2 * hh2 + q2
                                P.add(PE, (lambda e, q=q, q2=q2, n=n: e.matmul(psb[4 + q][:n, :], lhsT=m_gt[:n, :n],
                                                                              rhs=rhsA[:n, 4 * q2:4 * q2 + 4, :].rearrange("p h l -> p (h l)"),
                                                                              start=True, stop=True)),
                                      reads=["rhsA", "consts"], writes=[PK[4 + q]])
                                P.add(ACT, (lambda e, q=q, n=n: e.activation(out=Eexp[:n, 4 * q:4 * q + 4, :].rearrange("p h l -> p (h l)"),
                                                                            in_=psb[4 + q][:n, :], func=AF.Exp)),
                                      reads=[PK[4 + q]], writes=[PK[4 + q], "Eexp"])
                        def mcb(e, off=off, n=n):
                            ins = None
                            for g in range(4):
                                ins = e.matmul(psb[0][:n, g * 128:(g + 1) * 128], lhsT=xact[:, 8 + g, off:off + n], rhs=xact[:, 12 + g, off:off + n],
                                               start=True, stop=True)
                            return ins
                        P.add(PE, mcb, reads=["xact"], writes=[PK[0]])
                        P.add(DVE, (lambda e, n=n: e.tensor_tensor(out=cbm[:n], in0=psb[0][:n, :].rearrange("p (g l) -> p g l", g=4),
                                                                  in1=m_le[:n, :].unsqueeze(1).to_broadcast([n, 4, 128]), op=ALU.mult)),
                              reads=[PK[0], "consts"], writes=[PK[0], "cbm"])
                        P.add(DVE, (lambda e, n=n: e.tensor_tensor(out=WT[:n].rearrange("p (g r) l -> p g r l", g=4),
                                                                  in0=Eexp[:n].rearrange("p (g r) l -> p g r l", g=4),
                                                                  in1=cbm[:n].unsqueeze(2).to_broadcast([n, 4, 4, 128]), op=ALU.mult)),
                              reads=["Eexp", "cbm"], writes=["WT"])
                        for hf in range(2):
                            def myd(e, hf=hf, n=n):
                                ins = e.matmul(psb[4 + hf][:n, :], lhsT=ident_bf[:n, :n], rhs=xD[:n, 8 * hf:8 * hf + 8, :].rearrange("p h d -> p (h d)"),
                                               start=True, stop=False, skip_group_check=True)
                                for hh in range(8):
                                    h_ = 8 * hf + hh
                                    ins = e.matmul(psb[4 + hf][:n, hh * 64:(hh + 1) * 64], lhsT=WT[:n, h_, :n], rhs=xdt[:n, h_, :],
                                                   start=False, stop=(hh == 7), skip_group_check=True)
                                return ins
                            P.add(PE, myd, reads=["ident_bf", "xD", "WT", "xdt"], writes=[PK[4 + hf]])

                            def myo(e, hf=hf, off=off, n=n):
                                ins = None
                                for gg in range(2):
                                    g = 2 * hf + gg
                                    ins = e.matmul(psb[6 + hf][:n, gg * 256:(gg + 1) * 256], lhsT=xact[:, 12 + g, off:off + n],
                                                   rhs=state_bf[:, 4 * g:4 * g + 4, :].rearrange("p h d -> p (h d)"), start=True, stop=True)
                                return ins
                            P.add(PE, myo, reads=["xact", "state_bf"], writes=[PK[6 + hf]])
                    for hf in range(2):
                        def mst(e, hf=hf, n=n):
                            ins = None
                            for gg in range(2):
                                g = 2 * hf + gg
                                ins = e.matmul(psb[2 + hf][:, gg * 256:(gg + 1) * 256], lhsT=B_tm[:n, g * 128:(g + 1) * 128],
                                               rhs=xdd[:n, 4 * g:4 * g + 4, :].rearrange("p h d -> p (h d)"), start=True, stop=True)
                            return ins
                        P.add(PE, mst, reads=["B_tm", "xdd"], writes=[PK[2 + hf]])
                    P.add(DVE, (lambda e: e.tensor_tensor(out=state, in0=state, in1=cdec[:, :].unsqueeze(2).to_broadcast([128, 16, 64]), op=ALU.mult)),
                          reads=["state", "cdec"], writes=["state"])
                    for hf in range(2):
                        P.add(DVE, (lambda e, hf=hf: e.tensor_tensor(out=state[:, 8 * hf:8 * hf + 8, :].rearrange("p h d -> p (h d)"),
                                                                    in0=state[:, 8 * hf:8 * hf + 8, :].rearrange("p h d -> p (h d)"),
                                                                    in1=psb[2 + hf][:, :], op=ALU.add)),
                              reads=["state", PK[2 + hf]], writes=[PK[2 + hf], "state"])
                    P.add(ACT, (lambda e: e.copy(out=state_bf, in_=state)), reads=["state"], writes=["state_bf"])
                    if real:
                        for hf in range(2):
                            P.add(DVE, (lambda e, hf=hf, n=n, t1=t1: e.tensor_tensor(
                                out=t1[:n, hf * 512:(hf + 1) * 512].rearrange("p (h d) -> p h d", h=8),
                                in0=psb[6 + hf][:n, :].rearrange("p (h d) -> p h d", h=8),
                                in1=expcs[:n, 8 * hf:8 * hf + 8].unsqueeze(2).to_broadcast([n, 8, 64]), op=ALU.mult)),
                                reads=[PK[6 + hf], "expcs"], writes=[PK[6 + hf], f"t1{hf}_{cp}"])
                            P.add(DVE, (lambda e, hf=hf, n=n, t1=t1: e.tensor_tensor(out=t1[:n, hf * 512:(hf + 1) * 512], in0=t1[:n, hf * 512:(hf + 1) * 512],
                                                                             in1=psb[4 + hf][:n, :], op=ALU.add)),
                                  reads=[PK[4 + hf], f"t1{hf}_{cp}"], writes=[PK[4 + hf], f"t1b{hf}_{cp}"])
                        def tail(n=n, ti=ti, cp=cp, sz=sz, t1=t1, gy=gy, szk=szk):
                            gk = f"gy_{cp}"
                            P.add(DVE, (lambda e: e.tensor_tensor(out=gy[:n], in0=t1[:n], in1=sz[:n], op=ALU.mult)),
                                  reads=[f"t1b0_{cp}", f"t1b1_{cp}", szk], writes=[gk])
                            for g in range(4):
                                P.add(ACT, (lambda e, g=g: e.activation(out=junk[:n], in_=gy[:n, g * 256:(g + 1) * 256], func=AF.Square,
                                                                       accum_out=ss[:n, g:g + 1])),
                                      reads=[gk], writes=["junk", f"ss{g}"])
                            P.add(ACT, (lambda e: e.activation(out=rstd4[:n], in_=ss[:n], func=AF.Ln, bias=rmseps_t[:n], scale=1.0 / 256)),
                                  reads=[f"ss{g}" for g in range(4)] + ["eps"], writes=["rstd4a"])
                            P.add(ACT, (lambda e: e.activation(out=rstd4[:n], in_=rstd4[:n], func=AF.Exp, scale=-0.5)),
                                  reads=["rstd4a"], writes=["rstd4"])
                            for g in range(4):
                                P.add(DVE, (lambda e, g=g: e.scalar_tensor_tensor(out=gout[:n, g * 256:(g + 1) * 256], in0=gy[:n, g * 256:(g + 1) * 256],
                                                                                 scalar=rstd4[:n, g:g + 1], in1=nw_bc[:n, g * 256:(g + 1) * 256],
                                                                                 op0=ALU.mult, op1=ALU.mult)),
                                      reads=[gk, "rstd4", "nw"], writes=["gout"])
                            r0 = (ti - 1) * 128
                            P.add(SP, (lambda e: e.dma_start(out=g_scr[r0:r0 + 128, :], in_=gout[:, :])), reads=["gout"], writes=[f"gscr{ti}"], dma=True)
                            if debug == "gssd":
                                P.add(SP, (lambda e: e.dma_start(out=dbgb_d[16 + r0:16 + r0 + 128, :], in_=gout[:, :])), reads=["gout"], writes=["dbg"], dma=True)
                        while tailq:
                            tailq.pop(0)()
                        tailq.append(tail)
            while tailq:
                tailq.pop(0)()
            barrier()


        if stop_after >= 2:
            AR.reset(H0T_WORDS)
            stage = None
            Wh = AR.alloc([128, 8, 384], BF16)
            QT = AR.alloc([128, T], BF16)
            KT = AR.alloc([128, T], BF16)
            Vaug = AR.alloc([128, NTILE, 130], BF16)
            PT = [AR.alloc([128, 2, 512], BF16) for _ in range(3)]
            attc_f = AR.alloc([128, 1024])
            attc = AR.alloc([128, 1024], BF16)
            P.add(SP, lambda e: e.dma_start(out=attc_f[0:1, :], in_=attc_d), writes=["attc_f"], dma=True)
            P.add(DVE, lambda e: e.tensor_copy(out=attc[0:1, :], in_=attc_f[0:1, :]), reads=["attc_f"], writes=["attc"])
            maskk = attc[0:1, 0:128]
            onesr = attc[0:1, 128:640]
            zeror = attc[0:1, 640:1024]
            lamv = AR.alloc([128, 4, 64])
            for i4 in range(4):
                bcast_load(lamv[:, i4, :], lam_d[i4:i4 + 1, :], "lamv")
            lsm = AR.alloc([128, 16])
            ljunk = AR.alloc([128, 64])
            for i2 in range(2):
                P.add(DVE, (lambda e, i2=i2: e.scalar_tensor_tensor(out=ljunk, in0=lamv[:, 2 * i2, :], scalar=1.0, in1=lamv[:, 2 * i2 + 1, :],
                                                                   op0=ALU.mult, op1=ALU.mult, accum_out=lsm[:, i2:i2 + 1])),
                      reads=["lamv"], writes=["ljunk", f"lsm{i2}"])
            P.add(ACT, lambda e: e.activation(out=lsm[:, 2:4], in_=lsm[:, 0:2], func=AF.Exp), reads=["lsm0", "lsm1"], writes=["lsme"])
            P.add(DVE, lambda e: e.tensor_tensor(out=lsm[:, 4:5], in0=lsm[:, 3:4], in1=lsm[:, 2:3], op=ALU.subtract), reads=["lsme"], writes=["nl0"])
            P.add(DVE, lambda e: e.tensor_scalar(out=lsm[:, 5:6], in0=lsm[:, 4:5], scalar1=-LAMBDA_INIT, scalar2=None, op0=ALU.add), reads=["nl0"], writes=["neglam"])
            neglam = lsm[:, 5:6]
            subw = AR.alloc([128, 128])
            bcast_load(subw, subln_d, "subw0")
            P.add(DVE, lambda e: e.tensor_scalar(out=subw, in0=subw, scalar1=1.0 - LAMBDA_INIT, scalar2=None, op0=ALU.mult), reads=["subw0"], writes=["subw"])
            accsb = [AR.alloc([128, 8, 130]) for _ in range(2)]
            fin = [AR.alloc([128, 16]) for _ in range(2)]
            oq = [AR.alloc([128, 4, 128]) for _ in range(2)]
            osq = [AR.alloc([128, 4, 128]) for _ in range(2)]
            osb = [AR.alloc([128, 4, 128], BF16) for _ in range(2)]
            pending = []
            gpend = []
            zt = AR.alloc([128, 4096], BF16)
            ztf = zt.bitcast(F32)
            P.add(POOL, lambda e: e.memset(zt, 0.0), writes=["z_zt"])
            xs_flat = xs_scr.rearrange("s d -> (s d)").rearrange("(p f) -> p f", p=128)
            acc_flat = acc_scr.rearrange("s d -> (s d)").rearrange("(p f) -> p f", p=128)
            meta_flat = meta_scr.rearrange("s d -> (s d)").rearrange("(p f) -> p f", p=128)
            nacc = (SEQ + 128) * D // 128
            zfills = []
            for j in range(NSLOT * D // 128 // 4096):
                zfills.append((lambda j=j: P.add(POOL, (lambda e: e.dma_start(out=xs_flat[:, j * 4096:(j + 1) * 4096], in_=zt)), reads=["z_zt"], writes=[f"zf_xs{j}"], dma=True)))
            for j0 in range(0, nacc, 2048):
                w_ = min(2048, nacc - j0)
                zfills.append((lambda j0=j0, w_=w_: P.add(POOL, (lambda e: e.dma_start(out=acc_flat[:, j0:j0 + w_], in_=ztf[:, 0:w_])), reads=["z_zt"], writes=[f"zf_acc{j0}"], dma=True)))
            zfills.append((lambda: P.add(POOL, lambda e: e.dma_start(out=meta_flat, in_=ztf[:, 0:NSLOT * 2 // 128]), reads=["z_zt"], writes=["zf_meta"], dma=True)))
            P.add(DVE, lambda e: e.memset(Vaug[:, :, 128:130], 1.0), writes=["Vaug1"])
            Wg = AR.alloc([128, 8, 2048], BF16)
            load_cast(Wg, w_in_d[:, C_G:C_G + 2048], 2048, "Wg", stage)
            bgate = AR.alloc([128, 2048], BF16)
            P.add(POOL, lambda e: e.dma_start(out=bgate[0:1, :], in_=bgate_d), writes=["bgate"], dma=True)
            sgt = [AR.alloc([128, 2048], BF16) for _ in range(2)]
            sge = [AR.alloc([128, 512]) for _ in range(2)]
            blocks = [(0, 16)] + [(16 + 512 * g, 512) for g in range(8)]
            it = 0
            for h in range(8):
                for pi, cbase in enumerate((C_Q, C_K, C_V)):
                    load_cast(Wh[:, :, pi * 128:(pi + 1) * 128], w_in_d[:, cbase + h * 128:cbase + (h + 1) * 128], 128, "Wh", stage)
                for _z in range((len(zfills) + 7 - h) // (8 - h) if h < 7 else len(zfills)):
                    if zfills:
                        zfills.pop(0)()
                for which, dst, scale in ((0, QT, 0.125), (1, KT, 1.0)):
                    for (bc0, N) in blocks:
                        if which == 0 and bc0 == 0:
                            continue
                        def mp(e, which=which, bc0=bc0, N=N):
                            ins = None
                            for kc in range(8):
                                ins = e.matmul(psb[7][:, 0:N], lhsT=Wh[:, kc, which * 128:(which + 1) * 128], rhs=h0T[:, kc, bc0:bc0 + N],
                                               start=(kc == 0), stop=(kc == 7))
                            return ins
                        P.add(PE, mp, reads=["Wh"] + H0T_ALL, writes=[PK[7]])
                        P.add(DVE, (lambda e, dst=dst, bc0=bc0, N=N, scale=scale: e.tensor_scalar(
                            out=dst[:, bc0:bc0 + N], in0=psb[7][:, 0:N], scalar1=scale, scalar2=None, op0=ALU.mult)),
                            reads=[PK[7]], writes=[PK[7], "QT" if which == 0 else "KT"])
                for t0 in range(0, NTILE, 4):
                    tl = list(range(t0, min(NTILE, t0 + 4)))
                    def mv(e, tl=tl):
                        ins = None
                        for si, ti in enumerate(tl):
                            tc0, n = tcol(ti)
                            for kc in range(8):
                                ins = e.matmul(psb[7][:n, si * 128:(si + 1) * 128], lhsT=h0T[:, kc, tc0:tc0 + n], rhs=Wh[:, kc, 256:384],
                                               start=(kc == 0), stop=(kc == 7))
                        return ins
                    P.add(PE, mv, reads=["Wh"] + H0T_ALL, writes=[PK[7]])
                    nt = len(tl)
                    if t0 == 0:
                        P.add(DVE, (lambda e: e.tensor_copy(out=Vaug[:16, 0, 0:128], in_=psb[7][:16, 0:128])), reads=[PK[7]], writes=[PK[7], "Vaug"])
                        P.add(DVE, (lambda e, nt=nt: e.tensor_copy(out=Vaug[:, 1:nt, 0:128], in_=psb[7][:, 128:nt * 128].rearrange("p (t c) -> p t c", c=128))),
                              reads=[PK[7]], writes=[PK[7], "Vaug"])
                    else:
                        P.add(DVE, (lambda e, t0=t0, nt=nt: e.tensor_copy(out=Vaug[:, t0:t0 + nt, 0:128], in_=psb[7][:, 0:nt * 128].rearrange("p (t c) -> p t c", c=128))),
                              reads=[PK[7]], writes=[PK[7], "Vaug"])
                for ti in range(4 * h + 1, 4 * h + 5):
                    tc0, n = tcol(ti)
                    sb2 = ti % 2
                    for cc in range(4):
                        def gunit(tc0=tc0, cc=cc, sb2=sb2, ti=ti):
                            def mg(e):
                                for kc in range(8):
                                    e.matmul(psb[7][:, :], lhsT=h0T[:, kc, tc0:tc0 + 128], rhs=Wg[:, kc, cc * 512:(cc + 1) * 512],
                                             start=(kc == 0), stop=False)
                                return e.matmul(psb[7][:, :], lhsT=onesr[0:1, 0:128], rhs=bgate[0:1, cc * 512:(cc + 1) * 512], start=False, stop=True)
                            P.add(PE, mg, reads=["Wg", "bgate", "attc", f"h0T{ti}"], writes=[PK[7]])
                            eb = cc % 2
                            P.add(ACT, (lambda e: e.activation(out=sge[eb], in_=psb[7][:, :], func=AF.Exp, scale=-1.0)),
                                  reads=[PK[7]], writes=[PK[7], f"sge{eb}"])
                            P.add(DVE, (lambda e: e.tensor_scalar(out=sge[eb], in0=sge[eb], scalar1=1.0, scalar2=None, op0=ALU.add)),
                                  reads=[f"sge{eb}"], writes=[f"sge{eb}"])
                            P.add(DVE, (lambda e: e.reciprocal(out=sgt[sb2][:, cc * 512:(cc + 1) * 512], in_=sge[eb])),
                                  reads=[f"sge{eb}"], writes=[f"sgt{sb2}"])
                            if cc == 3:
                                P.add(SP, (lambda e: e.dma_start(out=sg_scr[(ti - 1) * 128:ti * 128, :], in_=sgt[sb2])),
                                      reads=[f"sgt{sb2}"], writes=[f"sgscr{ti}"], dma=True)
                        gpend.append(gunit)
                for G in range(8):
                    qc0 = 16 + 512 * G
                    gidx = h * 8 + G
                    ab = gidx % 2

                    def acc(qi, j):
                        a_ = qi * 2 + j
                        return psb[4 + a_ // 3][:, (a_ % 3) * 130:(a_ % 3) * 130 + 130]
                    klist = [("m", 0)] + [("r", kt) for kt in range(4 * G + 4)]
                    infos = []
                    for (kind, kt) in klist:
                        sb_ = 2 * (it % 2)
                        pts = it % 3
                        it += 1
                        if kind == "m":
                            kc0, kn, qi0, vt = 0, 16, 0, 0
                        else:
                            kc0, kn, qi0, vt = 16 + 128 * kt, 128, max(0, kt - 4 * G), 1 + kt
                        N = 128 * (4 - qi0)
                        q0 = qc0 + 128 * qi0
                        diag = (kind == "r" and kt >= 4 * G)
                        infos.append((sb_, pts, kc0, kn, qi0, vt, N, q0, diag))

                    def emit_scores(info):
                        sb_, pts, kc0, kn, qi0, vt, N, q0, diag = info

                        def ms(e):
                            ins = None
                            for j in range(2):
                                ins = e.matmul(psb[sb_ + j][:kn, 0:N], lhsT=KT[64 * j:64 * j + 64, kc0:kc0 + kn], rhs=QT[64 * j:64 * j + 64, q0:q0 + N],
                                               start=True, stop=True, skip_group_check=True)
                            if diag:
                                for j in range(2):
                                    ins = e.matmul(psb[sb_ + j][:, 0:64], lhsT=maskk, rhs=onesr[0:1, 0:64], start=False, stop=True, skip_group_check=True)
                            return ins
                        P.add(PE, ms, reads=["QT", "KT", "attc"], writes=[PK[sb_], PK[sb_ + 1]])
                        P.add(ACT, (lambda e: e.activation(
                            out=PT[pts][:kn, :, 0:N], in_=psall[:kn, sb_ * 512:(sb_ + 2) * 512].rearrange("p (j n) -> p j n", j=2)[:, :, 0:N], func=AF.Exp)),
                            reads=[PK[sb_], PK[sb_ + 1]], writes=[PK[sb_], PK[sb_ + 1], f"PT{pts}"])

                    def emit_pv(info):
                        sb_, pts, kc0, kn, qi0, vt, N, q0, diag = info

                        def mpv(e):
                            ins = None
                            for qi in range(qi0, 4):
                                for j in range(2):
                                    ins = e.matmul(acc(qi, j), lhsT=PT[pts][:kn, j, (qi - qi0) * 128:(qi - qi0 + 1) * 128], rhs=Vaug[:kn, vt, :],
                                                   start=False, stop=True, skip_group_check=True)
                            return ins
                        P.add(PE, mpv, reads=[f"PT{pts}", "Vaug", "Vaug1"], writes=[PK[4], PK[5], PK[6]])

                    emit_scores(infos[0])
                    def zi(e):
                        ins = None
                        for b_ in (4, 5, 6):
                            ins = e.matmul(psb[b_][:, :], lhsT=zeror[0:1, 0:128], rhs=attc[0:1, 512:1024], start=True, stop=True, skip_group_check=True)
                        return ins
                    P.add(PE, zi, reads=["attc"], writes=[PK[4], PK[5], PK[6]])
                    for ii in range(len(infos)):
                        if ii + 1 < len(infos):
                            emit_scores(infos[ii + 1])
                        emit_pv(infos[ii])
                        if pending:
                            pending.pop(0)()
                        if gpend and ii % 2 == 1:
                            gpend.pop(0)()
                    acs = accsb[ab]
                    for b_ in range(3):
                        na = 3 if b_ < 2 else 2
                        P.add(DVE, (lambda e, b_=b_, na=na, acs=acs: e.tensor_copy(
                            out=acs[:, 3 * b_:3 * b_ + na, :].rearrange("p a c -> p (a c)"), in_=psb[4 + b_][:, 0:na * 130])),
                            reads=[PK[4 + b_]], writes=[PK[4 + b_], f"accsb{ab}"])

                    def fin1(ab=ab, acs=acs):
                        fk = f"fin{ab}"
                        F = fin[ab]
                        P.add(DVE, (lambda e: e.reciprocal(out=F[:, 0:8], in_=acs[:, :, 128])), reads=[f"accsb{ab}"], writes=[fk])
                        P.add(DVE, (lambda e: e.tensor_scalar(out=F[:, 0:8].rearrange("p (q j) -> p q j", j=2)[:, :, 1], in0=F[:, 0:8].rearrange("p (q j) -> p q j", j=2)[:, :, 1],
                                                              scalar1=neglam, scalar2=None, op0=ALU.mult)), reads=[fk, "neglam"], writes=[fk])
                        P.add(DVE, (lambda e: e.tensor_tensor(out=acs[:, :, 0:128], in0=acs[:, :, 0:128], in1=F[:, 0:8].unsqueeze(2).to_broadcast([128, 8, 128]), op=ALU.mult)),
                              reads=[f"accsb{ab}", fk], writes=[f"accsb{ab}"])
                        a4 = acs.rearrange("p (q j) c -> p q j c", j=2)
                        P.add(DVE, (lambda e: e.tensor_tensor(out=oq[ab], in0=a4[:, :, 0, 0:128], in1=a4[:, :, 1, 0:128], op=ALU.add)),
                              reads=[f"accsb{ab}"], writes=[f"oq{ab}"])
                        P.add(DVE, (lambda e: e.tensor_tensor(out=osq[ab], in0=oq[ab], in1=oq[ab], op=ALU.mult)), reads=[f"oq{ab}"], writes=[f"osq{ab}"])
                        P.add(DVE, (lambda e: e.tensor_reduce(out=F[:, 8:12], in_=osq[ab], axis=AX.X, op=ALU.add)), reads=[f"osq{ab}"], writes=[fk])

                    def fin2(ab=ab):
                        fk = f"fin{ab}"
                        F = fin[ab]
                        P.add(ACT, (lambda e: e.activation(out=F[:, 12:16], in_=F[:, 8:12], func=AF.Ln, bias=rmseps_t, scale=1.0 / 128)), reads=[fk, "eps"], writes=[fk])
                        P.add(ACT, (lambda e: e.activation(out=F[:, 12:16], in_=F[:, 12:16], func=AF.Exp, scale=-0.5)), reads=[fk], writes=[fk])

                    def fin3(ab=ab, G=G, h=h):
                        fk = f"fin{ab}"
                        F = fin[ab]
                        P.add(DVE, (lambda e: e.tensor_tensor(out=oq[ab], in0=oq[ab], in1=F[:, 12:16].unsqueeze(2).to_broadcast([128, 4, 128]), op=ALU.mult)),
                              reads=[f"oq{ab}", fk], writes=[f"oq{ab}"])
                        P.add(DVE, (lambda e: e.tensor_tensor(out=osb[ab], in0=oq[ab], in1=subw.unsqueeze(1).to_broadcast([128, 4, 128]), op=ALU.mult)),
                              reads=[f"oq{ab}", "subw"], writes=[f"osb{ab}"])
                        r0 = 512 * G
                        P.add(SP, (lambda e: e.dma_start(
                            out=o_scr[r0:r0 + 512, h * 128:(h + 1) * 128].rearrange("(q p) c -> p q c", p=128), in_=osb[ab])),
                            reads=[f"osb{ab}"], writes=[f"oscr{G}"], dma=True)
                        if debug == "oda":
                            P.add(SP, (lambda e: e.dma_start(
                                out=dbgb_d[16 + r0:16 + r0 + 512, h * 128:(h + 1) * 128].rearrange("(q p) c -> p q c", p=128), in_=osb[ab])),
                                reads=[f"osb{ab}"], writes=["dbg"], dma=True)
                    pending.extend([fin1, fin2, fin3])
            while pending:
                pending.pop(0)()
            while gpend:
                gpend.pop(0)()
            barrier()

        if stop_after >= 3:
            AR.reset(0)
            stage = None
            Wso = AR.alloc([128, 8, D], BF16)
            Wdo = AR.alloc([128, 8, D], BF16)
            Wo = AR.alloc([128, 8, D], BF16)
            load_cast(Wso, wso_d, D, "Wso", stage)
            load_cast(Wdo, wdo_d, D, "Wdo", stage)
            load_cast(Wo, wo_d, D, "Wo", stage)
            l1g = AR.alloc([128, D])
            l1b = AR.alloc([128, D])
            bcast_load(l1g, ln1g_d, "l1g")
            bcast_load(l1b, ln1b_d, "l1b")
            wr = AR.alloc([128, 8, NEXP])
            P.add(SP, lambda e: e.dma_start(out=wr, in_=wr_d.rearrange("(k p) c -> p k c", p=128)), writes=["wr"], dma=True)
            brt = AR.alloc([128, NEXP])
            P.add(SP, lambda e: e.dma_start(out=brt[0:1, :], in_=br_d), writes=["brt"], dma=True)
            cnt_eb = AR.alloc([128, NEXP])
            P.add(DVE, lambda e: e.tensor_copy(out=cnt_eb, in_=ebase), reads=["consts"], writes=["cnt_eb"])
            NB3 = 3
            NL3 = 4
            gt = [AR.alloc([128, D], BF16) for _ in range(NL3)]
            ot = [AR.alloc([128, D], BF16) for _ in range(NL3)]
            sgl = [AR.alloc([128, 2 * D], BF16) for _ in range(NL3)]
            rr = [AR.alloc([128, D]) for _ in range(NL3)]
            gT = [AR.alloc([128, 8, 128], BF16) for _ in range(NB3)]
            oT = [AR.alloc([128, 8, 128], BF16) for _ in range(NB3)]
            mT = [AR.alloc([128, 8, 128], BF16) for _ in range(NB3)]
            m1 = [AR.alloc([128, D]) for _ in range(NB3)]
            mg_ = [AR.alloc([128, D], BF16) for _ in range(NB3)]
            h1bf = [AR.alloc([128, D], BF16) for _ in range(NL3)]
            h1T = [AR.alloc([128, 4, 128]) for _ in range(2)]
            stat3 = [AR.alloc([128, 16]) for _ in range(NL3)]
            rt = [AR.alloc([128, 160]) for _ in range(NL3)]
            meta4 = [AR.alloc([128, 4, 2]) for _ in range(NL3)]
            for b_ in range(NL3):
                x_ = f"_{b_}"
                P.alias("mgbuf" + x_, "mgA0" + x_, "mgA1" + x_, "mg0" + x_, "mg1" + x_)
                P.alias("m1buf" + x_, "m10" + x_, "m11" + x_)
                x_ = f"_R{b_}"
                P.alias("rr" + x_, "rr0" + x_, "rr1" + x_)
                P.alias("rt" + x_ + "sb0", "rt" + x_ + "sb")
                P.alias("rt" + x_ + "gsum", "rt" + x_ + "grs")

            def tr_to(src, dst, skey, dkey, bank, eng_copy):
                def trf(e, src=src, bank=bank):
                    ins = None
                    pv = psbf(bank)
                    for c in range(8):
                        ins = e.transpose(out=pv[:, c * 128:(c + 1) * 128], in_=src[:, c * 128:(c + 1) * 128], identity=ident_bf[:, :])
                    return ins
                P.add(PE, trf, reads=(skey if isinstance(skey, list) else [skey]) + ["ident_bf"], writes=[PK[bank]])
                if eng_copy == ACT:
                    P.add(ACT, (lambda e, dst=dst, bank=bank: e.copy(out=dst.rearrange("p k t -> p (k t)"), in_=psbf(bank))),
                          reads=[PK[bank]], writes=[PK[bank], dkey])
                else:
                    P.add(DVE, (lambda e, dst=dst, bank=bank: e.tensor_copy(out=dst.rearrange("p k t -> p (k t)"), in_=psbf(bank))),
                          reads=[PK[bank]], writes=[PK[bank], dkey])

            def lin(actT, W, akey, wkey, banks):
                for hf in range(2):
                    def ml(e, actT=actT, W=W, hf=hf, bank=banks[hf]):
                        ins = None
                        for kc in range(8):
                            ins = e.matmul(psb[bank][:, :], lhsT=actT[:, kc, :], rhs=W[:, kc, hf * 512:(hf + 1) * 512], start=(kc == 0), stop=(kc == 7))
                        return ins
                    P.add(PE, ml, reads=[akey, wkey], writes=[PK[banks[hf]]])

            def loads3(i):
                lb = i % NL3
                lx = f"_L{lb}"
                ti = i + 1
                r0 = 128 * i
                P.add(SP, (lambda e: e.dma_start(out=gt[lb], in_=g_scr[r0:r0 + 128, :])), reads=[f"gscr{ti}"], writes=["gt" + lx], dma=True)
                P.add(SP, (lambda e: e.dma_start(out=ot[lb], in_=o_scr[r0:r0 + 128, :])), reads=[f"oscr{i // 4}"], writes=["ot" + lx], dma=True)
                P.add(SP, (lambda e: e.dma_start(out=sgl[lb], in_=sg_scr[r0:r0 + 128, :])), reads=[f"sgscr{ti}"], writes=["sgl" + lx], dma=True)

            def front3(i):
                b = i % NB3
                lb = i % NL3
                sfx = f"_{b}"
                lx = f"_L{lb}"
                ti = i + 1
                tc0 = 16 + 128 * i
                P.add(SP, (lambda e: e.dma_start(out=rr[lb], in_=h0_scr[tc0:tc0 + 128, :])), reads=[f"h0s{ti}"], writes=["rr" + f"_R{lb}"], dma=True)
                tr_to(gt[lb], gT[b], "gt" + lx, "gT" + sfx, 0, DVE)
                tr_to(ot[lb], oT[b], "ot" + lx, "oT" + sfx, 1, DVE)
                lin(gT[b], Wso, "gT" + sfx, "Wso", (2, 3))
                for hf in range(2):
                    P.add(DVE, (lambda e, hf=hf: e.tensor_tensor(out=m1[b][:, hf * 512:(hf + 1) * 512], in0=psb[2 + hf][:, :],
                                                                in1=sgl[lb][:, hf * 512:(hf + 1) * 512], op=ALU.mult)),
                          reads=[PK[2 + hf], "sgl" + lx], writes=[PK[2 + hf], f"m1{hf}" + sfx])
                lin(oT[b], Wdo, "oT" + sfx, "Wdo", (4, 5))
                for hf in range(2):
                    P.add(DVE, (lambda e, hf=hf: e.tensor_tensor(out=mg_[b][:, hf * 512:(hf + 1) * 512], in0=psb[4 + hf][:, :],
                                                                in1=sgl[lb][:, D + hf * 512:D + (hf + 1) * 512], op=ALU.mult)),
                          reads=[PK[4 + hf], "sgl" + lx], writes=[PK[4 + hf], f"mgA{hf}" + sfx])
                    P.add(DVE, (lambda e, hf=hf: e.tensor_tensor(out=mg_[b][:, hf * 512:(hf + 1) * 512], in0=mg_[b][:, hf * 512:(hf + 1) * 512],
                                                                 in1=m1[b][:, hf * 512:(hf + 1) * 512], op=ALU.add)),
                          reads=[f"mgA{hf}" + sfx, f"m1{hf}" + sfx], writes=[f"mg{hf}" + sfx])

            def back_a(i):
                b = i % NB3
                rb = i % NL3
                sfx = f"_{b}"
                rx = f"_R{rb}"
                r0 = 128 * i
                if debug == "merged":
                    P.add(SP, (lambda e: e.dma_start(out=dbgb_d[16 + r0:16 + r0 + 128, :], in_=mg_[b])), reads=["mg0" + sfx, "mg1" + sfx], writes=["dbg"], dma=True)
                tr_to(mg_[b], mT[b], ["mg0" + sfx, "mg1" + sfx], "mT" + sfx, 0, DVE)
                lin(mT[b], Wo, "mT" + sfx, "Wo", (6, 7))
                for hf in range(2):
                    P.add(DVE, (lambda e, hf=hf: e.scalar_tensor_tensor(out=rr[rb][:, hf * 512:(hf + 1) * 512], in0=rr[rb][:, hf * 512:(hf + 1) * 512], scalar=ALPHA,
                                                                       in1=psb[6 + hf][:, :], op0=ALU.mult, op1=ALU.add)),
                          reads=[PK[6 + hf], "rr" + rx], writes=[PK[6 + hf], f"rr{hf}" + rx])
                layer_norm_tile(rr[rb], 128, l1g, l1b, rr[rb], ["rr0" + rx, "rr1" + rx], "h1" + rx, f"ln1{rb}", stat3[rb], ["l1g", "l1b"], eng2=DVE)
                hk = "h1" + rx
                P.add(SP, (lambda e: e.dma_start(out=h1_scr[r0:r0 + 128, :], in_=rr[rb])), reads=[hk], writes=[f"h1s{i}"], dma=True)
                if debug == "h1":
                    P.add(SP, (lambda e: e.dma_start(out=dbg_d[16 + r0:16 + r0 + 128, :], in_=rr[rb])), reads=[hk], writes=["dbg"], dma=True)
                P.add(POOL, (lambda e: e.tensor_copy(out=h1bf[rb], in_=rr[rb])), reads=[hk], writes=["h1bf" + rx])

            def back_b(i):
                rb = i % NL3
                rx = f"_R{rb}"
                sfx = rx
                b = rb
                r0 = 128 * i
                hk = "h1" + rx
                for hf in range(2):
                    def trh(e, hf=hf):
                        ins = None
                        for c in range(4):
                            ins = e.transpose(out=psb[6 + hf][:, c * 128:(c + 1) * 128], in_=rr[rb][:, (4 * hf + c) * 128:(4 * hf + c + 1) * 128], identity=ident)
                        return ins
                    P.add(PE, trh, reads=[hk, "consts"], writes=[PK[6 + hf]])
                    P.add(ACT, (lambda e, hf=hf: e.copy(out=h1T[hf].rearrange("p k t -> p (k t)"), in_=psb[6 + hf][:, :])), reads=[PK[6 + hf]], writes=[PK[6 + hf], f"h1T{hf}"])

                    def mr(e, hf=hf):
                        ins = None
                        for c in range(4):
                            ins = e.matmul(psb[1][:, 0:NEXP], lhsT=h1T[hf][:, c, :], rhs=wr[:, 4 * hf + c, :], start=(hf == 0 and c == 0), stop=False)
                        if hf == 1:
                            ins = e.matmul(psb[1][:, 0:NEXP], lhsT=ones[0:1, :], rhs=brt[0:1, :], start=False, stop=True)
                        return ins
                    P.add(PE, mr, reads=[f"h1T{hf}", "wr", "brt", "consts"], writes=[PK[1]])
                R = rt[b]
                lg, v8, msk, sbase = R[:, 0:32], R[:, 32:40], R[:, 40:72], R[:, 72:104]
                nv0, ex4, gsum, destf, j32 = R[:, 104:105], R[:, 105:109], R[:, 109:110], R[:, 110:114], R[:, 128:160]
                rk = "rt" + sfx
                P.add(DVE, (lambda e: e.tensor_copy(out=lg, in_=psb[1][:, 0:NEXP])), reads=[PK[1]], writes=[PK[1], rk + "lg"])
                if debug == "logits":
                    P.add(SP, (lambda e: e.dma_start(out=dbg_d[16 + r0:16 + r0 + 128, 0:32], in_=lg)), reads=[rk + "lg"], writes=["dbg"], dma=True)
                P.add(DVE, (lambda e: e.max(out=v8, in_=lg)), reads=[rk + "lg"], writes=[rk + "v8"])
                P.add(DVE, (lambda e: e.tensor_scalar(out=msk, in0=lg, scalar1=v8[:, 3:4], scalar2=None, op0=ALU.is_ge)),
                      reads=[rk + "lg", rk + "v8"], writes=[rk + "msk"])

                def mpos(e):
                    e.matmul(psb[1][:, 64:96], lhsT=m_lt, rhs=msk, start=True, stop=True)
                    return e.matmul(psb[1][:, 96:128], lhsT=ones, rhs=msk, start=True, stop=True)
                P.add(PE, mpos, reads=[rk + "msk", "consts"], writes=[PK[1]])
                P.add(DVE, (lambda e: e.tensor_tensor(out=sbase, in0=psb[1][:, 64:96], in1=cnt_eb, op=ALU.add)),
                      reads=[PK[1], "cnt_eb"], writes=[PK[1], rk + "sb0"])
                P.add(DVE, (lambda e: e.tensor_tensor(out=sbase, in0=sbase, in1=emax, op=ALU.min)), reads=[rk + "sb0", "consts"], writes=[rk + "sb"])
                P.add(DVE, (lambda e: e.tensor_tensor(out=cnt_eb, in0=psb[1][:, 96:128], in1=cnt_eb, op=ALU.add)), reads=[PK[1], "cnt_eb"], writes=[PK[1], "cnt_eb"])
                P.add(DVE, (lambda e: e.tensor_scalar(out=nv0, in0=v8[:, 0:1], scalar1=-1.0, scalar2=None, op0=ALU.mult)), reads=[rk + "v8"], writes=[rk + "nv0"])
                P.add(ACT, (lambda e: e.activation(out=ex4, in_=v8[:, 0:4], func=AF.Exp, bias=nv0, scale=1.0, accum_out=gsum)),
                      reads=[rk + "v8", rk + "nv0"], writes=[rk + "ex4", rk + "gsum"])
                P.add(DVE, (lambda e: e.reciprocal(out=gsum, in_=gsum)), reads=[rk + "gsum"], writes=[rk + "grs"])
                P.add(DVE, (lambda e: e.tensor_scalar(out=gate_all[:, i, :], in0=ex4, scalar1=gsum, scalar2=None, op0=ALU.mult)),
                      reads=[rk + "ex4", rk + "grs"], writes=[f"gate{i}"])
                for k in range(4):
                    P.add(DVE, (lambda e, k=k: e.scalar_tensor_tensor(
                        out=j32, in0=lg, scalar=v8[:, k:k + 1], in1=sbase, op0=ALU.is_equal, op1=ALU.mult, accum_out=destf[:, k:k + 1])),
                        reads=[rk + "lg", rk + "v8", rk + "sb"], writes=[rk + "j32", rk + f"df{k}"])
                P.add(DVE, (lambda e: e.tensor_copy(out=dest_all[:, i, :], in_=destf)), reads=[rk + f"df{k}" for k in range(4)], writes=[f"dest{i}"])
                M4 = meta4[b]
                P.add(DVE, (lambda e: e.tensor_scalar(out=M4[:, :, 0], in0=vcol.to_broadcast([128, 4]), scalar1=float(-128 * i), scalar2=None, op0=ALU.add)),
                      reads=["consts"], writes=["meta4" + sfx])
                P.add(DVE, (lambda e: e.tensor_copy(out=M4[:, :, 1], in_=gate_all[:, i, :])), reads=[f"gate{i}"], writes=["meta4" + sfx])
                for k in range(4):
                    P.add(POOL, (lambda e, k=k: e.indirect_dma_start(out=xs_scr, out_offset=IndirectOffsetOnAxis(ap=dest_all[:, i, k:k + 1], axis=0),
                                                                    in_=h1bf[b][:, :], in_offset=None)),
                          reads=[f"dest{i}", "h1bf" + sfx], writes=[f"xs{i}_{k}"], dma=True)
                    P.add(POOL, (lambda e, k=k: e.indirect_dma_start(out=meta_scr, out_offset=IndirectOffsetOnAxis(ap=dest_all[:, i, k:k + 1], axis=0),
                                                                    in_=M4[:, k, :], in_offset=None)),
                          reads=[f"dest{i}", "meta4" + sfx], writes=[f"xm{i}_{k}"], dma=True)

            loads3(0)
            loads3(1)
            loads3(2)
            front3(0)
            front3(1)
            for i in range(32):
                if i + 3 < 32:
                    loads3(i + 3)
                if i + 2 < 32:
                    front3(i + 2)
                if i >= 1:
                    back_b(i - 1)
                back_a(i)
            back_b(31)
            if debug == "route":
                P.add(SP, lambda e: e.dma_start(out=dbg_d[0:128, 0:128], in_=gate_all.rearrange("p a b -> p (a b)")), reads=[f"gate{i}" for i in range(32)], writes=["dbg"], dma=True)
                dtmp = AR.alloc([128, 128])
                P.add(DVE, lambda e: e.tensor_copy(out=dtmp, in_=dest_all.rearrange("p a b -> p (a b)")), reads=[f"dest{i}" for i in range(32)], writes=["dtmp"])
                P.add(SP, lambda e: e.dma_start(out=dbg_d[128:256, 0:128], in_=dtmp), reads=["dtmp"], writes=["dbg"], dma=True)
            barrier()

        if stop_after >= 4:
            AR.reset(0)
            NSTG = 4
            stage = [AR.alloc([128, 8, 256]) for _ in range(NSTG)]
            Wgu = [AR.alloc([128, 8, 2 * D], BF16) for _ in range(2)]
            Wd = [AR.alloc([128, 8, D], BF16) for _ in range(2)]
            bguT = AR.alloc([128, NEXP * 16])
            bguT1 = AR.alloc([128, NEXP * 16])
            P.add(SP, lambda e: e.dma_start(out=bguT, in_=bgu_d), writes=["bguT"], dma=True)
            P.add(DVE, lambda e: e.tensor_scalar(out=bguT1, in0=bguT, scalar1=1.0, scalar2=None, op0=ALU.add), reads=["bguT"], writes=["bguT1"])
            bdb = AR.alloc([128, D], BF16)
            P.add(DVE, lambda e: e.memset(bdb, 0.0), writes=["bdb"])
            P.add(POOL, lambda e: e.dma_start(out=bdb[0:NEXP, :], in_=bd_d), writes=["bdb"], dma=True)
            P.add(DVE, lambda e: e.tensor_scalar(out=bdb[0:NEXP, :], in0=bdb[0:NEXP, :], scalar1=1.702, scalar2=None, op0=ALU.mult), reads=["bdb"], writes=["bdb"])
            selb = AR.alloc([128, NEXP * 128], BF16)
            P.add(DVE, lambda e: e.memset(selb, 0.0), writes=["selb"])
            for q4 in range(4):
                P.add(POOL, (lambda e, q4=q4: e.dma_start(out=selb[0:NEXP, q4 * 1024:(q4 + 1) * 1024], in_=sel_d[:, q4 * 1024:(q4 + 1) * 1024])), writes=["selb"], dma=True)
            xsT = [AR.alloc([128, 8, CAP], BF16) for _ in range(2)]
            xtm = [AR.alloc([128, D], BF16) for _ in range(2)]
            actT = AR.alloc([128, 8, CAP], BF16)
            NBS = 2
            gbt = [AR.alloc([128, 512]) for _ in range(NBS)]
            sgm = [AR.alloc([128, 512]) for _ in range(NBS)]
            u1t = [AR.alloc([128, 512]) for _ in range(NBS)]
            yout = [AR.alloc([128, D], BF16) for _ in range(2)]
            mtl = [AR.alloc([128, 8]) for _ in range(3)]
            mti = [AR.alloc([128, 2], I32) for _ in range(3)]
            NBLK = CAP // 128
            xctr = [0]

            def weight_tasks(e_):
                wb = e_ % 2
                tasks = []
                chunks = []
                for q in range(8):
                    chunks.append((wgu_d, Wgu, q, 1, 2 * D, "Wgu"))
                for q in range(4):
                    chunks.append((wd_d, Wd, 2 * q, 2, D, "Wd"))
                binfo = {}

                def dma_part(k):
                    src, dstl, k0, nk, width, key = chunks[k]
                    bq = stg_ctr[0] % NSTG
                    stg_ctr[0] += 1
                    sv = stage[bq].rearrange("p k c -> p (k c)").rearrange("p (k c) -> p k c", k=nk)
                    binfo[k] = (bq, sv)
                    P.add(SP, (lambda e: e.dma_start(out=sv, in_=src[e_, k0 * 128:(k0 + nk) * 128, :].rearrange("(k p) c -> p k c", p=128))),
                          writes=[f"stg{bq}"], dma=True)

                def cast_part(k):
                    src, dstl, k0, nk, width, key = chunks[k]
                    bq, sv = binfo[k]
                    P.add(ACT, (lambda e: e.copy(out=dstl[wb][:, k0:k0 + nk, :], in_=sv)), reads=[f"stg{bq}"], writes=[f"{key}{wb}"])
                nch = len(chunks)
                for k in range(nch):
                    def task(k=k):
                        dma_part(k)
                        cast_part(k)
                    tasks.append(task)
                return tasks

            def prep_tasks(e_):
                xb = e_ % 2
                tasks = []
                for blk in range(NBLK):
                    def task(blk=blk, e_=e_, xb=xb):
                        tb = xctr[0] % 2
                        xctr[0] += 1
                        s0 = e_ * CAP + blk * 128
                        P.add(SP, (lambda e: e.dma_start(out=xtm[tb], in_=xs_scr[s0:s0 + 128, :])), writes=[f"xtm{tb}"], dma=True)

                        def trx(e):
                            ins = None
                            pv = psbf(0)
                            for c in range(8):
                                ins = e.transpose(out=pv[:, c * 128:(c + 1) * 128], in_=xtm[tb][:, c * 128:(c + 1) * 128], identity=ident_bf[:, :])
                            return ins
                        P.add(PE, trx, reads=[f"xtm{tb}", "ident_bf"], writes=[PK[0]])
                        P.add(DVE, (lambda e: e.tensor_copy(out=xsT[xb][:, :, blk * 128:(blk + 1) * 128], in_=psbf(0).rearrange("p (k t) -> p k t", k=8))),
                              reads=[PK[0]], writes=[PK[0], f"xsT{xb}"])
                    tasks.append(task)
                return tasks

            for t_ in weight_tasks(0) + prep_tasks(0):
                t_()
            dct = 0
            itc = 0
            for e_ in range(NEXP):
                wb = e_ % 2
                xb = e_ % 2
                bg_tasks = []
                if e_ + 1 < NEXP:
                    wt_, pt_ = weight_tasks(e_ + 1), prep_tasks(e_ + 1)
                    bg_tasks = wt_[0:4] + pt_[0:3] + wt_[4:8] + pt_[3:6] + wt_[8:12]
                for sgi, (s0, N) in enumerate(((0, 512), (512, CAP - 512))):
                    ak = f"actT{sgi}"
                    for c in range(8):
                        pa = 1 + 2 * (itc % 2)
                        tb = itc % NBS
                        itc += 1

                        def mgu(e, c=c, pa=pa, wb=wb, xb=xb, s0=s0, N=N):
                            ins = None
                            for half in range(2):
                                for kc in range(8):
                                    ins = e.matmul(psb[pa + half][:, 0:N], lhsT=Wgu[wb][:, kc, half * D + c * 128:half * D + (c + 1) * 128],
                                                   rhs=xsT[xb][:, kc, s0:s0 + N], start=(kc == 0), stop=(kc == 7))
                            return ins
                        P.add(PE, mgu, reads=[f"Wgu{wb}", f"xsT{xb}"], writes=[PK[pa], PK[pa + 1]])
                        P.add(DVE, (lambda e, pa=pa, tb=tb, N=N, e_=e_, c=c: e.tensor_scalar(out=gbt[tb][:, 0:N], in0=psb[pa][:, 0:N], scalar1=bguT[:, e_ * 16 + c:e_ * 16 + c + 1],
                                                                                             scalar2=7.0, op0=ALU.add, op1=ALU.min)),
                              reads=[PK[pa], "bguT"], writes=[PK[pa], f"gbt{tb}"])
                        P.add(ACT, (lambda e, tb=tb, N=N: e.activation(out=sgm[tb][:, 0:N], in_=gbt[tb][:, 0:N], func=AF.Silu, scale=1.702)),
                              reads=[f"gbt{tb}"], writes=[f"sgm{tb}"])
                        P.add(DVE, (lambda e, pa=pa, tb=tb, N=N, e_=e_, c=c: e.tensor_scalar(out=u1t[tb][:, 0:N], in0=psb[pa + 1][:, 0:N], scalar1=bguT1[:, e_ * 16 + 8 + c:e_ * 16 + 8 + c + 1],
                                                                                             scalar2=8.0, op0=ALU.add, op1=ALU.min)),
                              reads=[PK[pa + 1], "bguT1"], writes=[PK[pa + 1], f"u1t{tb}"])
                        P.add(DVE, (lambda e, tb=tb, N=N, c=c, s0=s0: e.scalar_tensor_tensor(out=actT[:, c, s0:s0 + N], in0=u1t[tb][:, 0:N], scalar=-6.0, in1=sgm[tb][:, 0:N],
                                                                                            op0=ALU.max, op1=ALU.mult)),
                              reads=[f"u1t{tb}", f"sgm{tb}"], writes=[ak])
                        if bg_tasks:
                            bg_tasks.pop(0)()
                    for blk in range(s0 // 128, (s0 + N) // 128):
                        yb = blk % 2
                        mb = dct % 3
                        sl0 = e_ * CAP + blk * 128
                        P.add(SP, (lambda e, mb=mb, sl0=sl0: e.dma_start(out=mtl[mb][:, 0:2], in_=meta_scr[sl0:sl0 + 128, :])), writes=[f"mtl{mb}"], dma=True)
                        P.add(DVE, (lambda e, mb=mb: e.tensor_scalar(out=mti[mb][:, 0:1], in0=mtl[mb][:, 0:1], scalar1=-1.0, scalar2=float(SEQ), op0=ALU.mult, op1=ALU.add)),
                              reads=[f"mtl{mb}"], writes=[f"mti{mb}"])
                        P.add(DVE, (lambda e, mb=mb: e.tensor_scalar(out=mtl[mb][:, 2:3], in0=mtl[mb][:, 1:2], scalar1=1.0 / 1.702, scalar2=None, op0=ALU.mult)),
                              reads=[f"mtl{mb}"], writes=[f"mtg{mb}"])
                        for half in range(2):
                            bank = 5 + dct % 3
                            dct += 1

                            def mdn(e, blk=blk, half=half, bank=bank, wb=wb, e_=e_):
                                for c in range(8):
                                    e.matmul(psb[bank][:, :], lhsT=actT[:, c, blk * 128:(blk + 1) * 128], rhs=Wd[wb][:, c, half * 512:(half + 1) * 512],
                                             start=(c == 0), stop=False)
                                return e.matmul(psb[bank][:, :], lhsT=selb[:, e_ * 128:(e_ + 1) * 128], rhs=bdb[:, half * 512:(half + 1) * 512], start=False, stop=True)
                            P.add(PE, mdn, reads=[ak, f"Wd{wb}", "selb", "bdb"], writes=[PK[bank]])
                            P.add(ACT, (lambda e, yb=yb, half=half, bank=bank, mb=mb: e.activation(out=yout[yb][:, half * 512:(half + 1) * 512], in_=psb[bank][:, :], func=AF.Copy,
                                                                                                     scale=mtl[mb][:, 2:3])),
                                  reads=[PK[bank], f"mtg{mb}"], writes=[PK[bank], f"yout{yb}"])
                        P.add(POOL, (lambda e, yb=yb, mb=mb: e.indirect_dma_start(out=acc_scr, out_offset=IndirectOffsetOnAxis(ap=mti[mb][:, 0:1], axis=0),
                                                                                 in_=yout[yb][:, :], in_offset=None, compute_op=ALU.add)),
                              reads=[f"yout{yb}", f"mti{mb}"], writes=["acc"], dma=True)
                        if bg_tasks:
                            bg_tasks.pop(0)()
                while bg_tasks:
                    bg_tasks.pop(0)()
            barrier()

        if stop_after >= 5:
            AR.reset(0)
            l2g = AR.alloc([128, D])
            l2b = AR.alloc([128, D])
            bcast_load(l2g, ln2g_d, "l2g")
            bcast_load(l2b, ln2b_d, "l2b")
            NB5 = 4
            hh = [AR.alloc([128, D]) for _ in range(NB5)]
            tt = [AR.alloc([128, D]) for _ in range(NB5)]
            stat5 = [AR.alloc([128, 16]) for _ in range(NB5)]
            for i in range(32):
                b = i % NB5
                sfx = f"_{b}"
                r0 = 128 * i
                P.add(SP, (lambda e, b=b, r0=r0: e.dma_start(out=hh[b], in_=h1_scr[r0:r0 + 128, :])), reads=[f"h1s{i}"], writes=["hh" + sfx], dma=True)
                P.add(SP, (lambda e, b=b, r0=r0: e.dma_start(out=tt[b], in_=acc_scr[r0:r0 + 128, :])), reads=["acc"], writes=["tt" + sfx], dma=True)
                P.add(DVE, (lambda e, b=b: e.scalar_tensor_tensor(out=hh[b], in0=hh[b], scalar=ALPHA, in1=tt[b], op0=ALU.mult, op1=ALU.add)),
                      reads=["hh" + sfx, "tt" + sfx], writes=["hh" + sfx])
                layer_norm_tile(hh[b], 128, l2g, l2b, hh[b], ["hh" + sfx], "ho" + sfx, f"ln2{b}", stat5[b], ["l2g", "l2b"], eng2=DVE, eng3=POOL)
                P.add(POOL, (lambda e, b=b, r0=r0: e.dma_start(out=out_d[r0:r0 + 128, :], in_=hh[b])), reads=["ho" + sfx], writes=[f"out{i}"], dma=True)

        sems = {}
        for e_ in (PE, ACT, DVE, POOL, SP):
            sems[e_] = es.enter_context(nc.semaphore(f"s_{e_}"))
        for e_ in (SP, POOL, ACT):
            for i in range(P.n_dma[e_]):
                sems[(e_, i)] = es.enter_context(nc.semaphore(f"d_{e_}{i}"))
        es.enter_context(nc.allow_low_precision(reason="bf16 matmul operands, fp32 accumulation"))
        block = es.enter_context(nc.Block())
        P.emit(block, sems)

        @block.tensor
        def _(eng):
            P._run(PE, eng)

        @block.scalar
        def _(eng):
            P._run(ACT, eng)

        @block.vector
        def _(eng):
            P._run(DVE, eng)

        @block.gpsimd
        def _(eng):
            P._run(POOL, eng)

        @block.sync
        def _(eng):
            P._run(SP, eng)
            final = {}
            for op in P.ops:
                if op.dma:
                    final[id(op.sem)] = (op.sem, max(final.get(id(op.sem), (None, 0))[1], op.val))
            for s, v in final.values():
                eng.wait_ge(s, v)
    return nc


def make_consts():
    k = np.arange(128)[:, None]
    l = np.arange(128)[None, :]
    c = np.zeros((128, 5 * 128 + 72), np.float32)
    c[:, 704] = SEQ - np.arange(128)
    c[:, 640:672] = np.arange(NEXP)[None, :] * CAP
    c[:, 672:704] = np.arange(NEXP)[None, :] * CAP + CAP - 1
    c[:, 0:128] = (k == l)
    c[:, 128:256] = (k <= l)
    c[:, 256:384] = (k > l)
    c[:, 384:512] = (k < l)
    c[:, 512:640] = 1.0
    return c


def make_sel():
    s_ = np.zeros((NEXP, NEXP, 128), np.float32)
    for e_ in range(NEXP):
        s_[e_, e_, :] = 1.0
    return s_.reshape(NEXP, NEXP * 128)


def make_attc():
    a = np.zeros((1, 1024), np.float32)
    a[0, 64:128] = -30000.0
    a[0, 128:640] = 1.0
    return a


def make_inputs(inputs, b):
    f = lambda a: np.ascontiguousarray(np.asarray(a, dtype=np.float32))
    cw = f(inputs["conv_w"])[0]
    cb = f(inputs["conv_b"])[0]
    return {
        "x": f(inputs["x"][b]),
        "meta": f(inputs["meta_tokens"]),
        "consts": make_consts(),
        "ln_in_g": f(inputs["ln_in_g"])[None],
        "ln_in_b": f(inputs["ln_in_b"])[None],
        "w_in": f(inputs["w_in"])[0],
        "convw_t": f(cw.reshape(4, 16, 128).transpose(2, 0, 1).reshape(128, 64)),
        "convb_t": f(cb.reshape(16, 128).T),
        "dt_bias": f(inputs["dt_bias"]),
        "a_log": f(inputs["a_log"]),
        "d_skip": f(inputs["d_skip"]),
        "ssd_norm_w": f(inputs["ssd_norm_w"]),
        "lam": f(np.concatenate([inputs["lam_q1"], inputs["lam_k1"], inputs["lam_q2"], inputs["lam_k2"]], axis=0)),
        "subln_w": f(inputs["subln_w"]),
        "attc": make_attc(),
        "b_gate": f(inputs["b_gate"]),
        "w_ssd_out": f(inputs["w_ssd_out"])[0],
        "w_da_out": f(inputs["w_da_out"])[0],
        "w_out": f(inputs["w_out"])[0],
        "ln1_g": f(inputs["ln1_g"]),
        "ln1_b": f(inputs["ln1_b"]),
        "w_router": f(inputs["w_router"])[0],
        "b_router": f(inputs["b_router"]),
        "w_gate_up": f(inputs["w_gate_up"])[0],
        "bgu_t": f(f(inputs["b_gate_up"])[0].reshape(NEXP, 16, 128).transpose(2, 0, 1).reshape(128, NEXP * 16)),
        "w_down": f(inputs["w_down"])[0],
        "b_down": f(inputs["b_down"])[0],
        "sel": make_sel(),
        "ln2_g": f(inputs["ln2_g"]),
        "ln2_b": f(inputs["ln2_b"]),
    }


_NC_CACHE = {}


def kernel(**inputs):
    n = 8
    if "nc" not in _NC_CACHE:
        _NC_CACHE["nc"] = build_program()
    nc = _NC_CACHE["nc"]
    shared = make_inputs(inputs, 0)
    in_maps = []
    for b in range(n):
        m = dict(shared)
        m["x"] = np.ascontiguousarray(np.asarray(inputs["x"][b], dtype=np.float32))
        in_maps.append(m)
    res = run_bass_kernel_spmd(nc, in_maps, core_ids=list(range(n)))
    out = np.stack([np.asarray(r["out"], dtype=np.float32) for r in res.results], axis=0)
    return out
```
